# Optimizing a Trainium2 kernel written in Bass

```python
import math
import jax, jax.numpy as jnp
from jax import lax
import numpy as np

D_MODEL = 1024
BATCH = 8
SEQ = 4096
DEPTH = 4

CHUNK = 64
POOL_WIDTH = D_MODEL // 2
POOL_WINDOWS = (2, 4, 8, 16)
N_POOL_GROUPS = len(POOL_WINDOWS)
POOL_GROUP = POOL_WIDTH // N_POOL_GROUPS
ATTN_WIDTH = D_MODEL - POOL_WIDTH
HEAD_DIM = 64
N_HEADS = ATTN_WIDTH // HEAD_DIM
ROPE_DIM = HEAD_DIM // 4
ROPE_THETA = 500000.0
IDX_HEADS = 8
IDX_DIM = 64
IDX_ROPE_DIM = IDX_DIM // 4
TOPK_MAX = 256
Q_BLOCK = 64
D_FF = 4 * D_MODEL
EPS = 1e-6

SPLITS = (POOL_WIDTH, ATTN_WIDTH, ATTN_WIDTH, ATTN_WIDTH, IDX_HEADS * IDX_DIM, IDX_DIM, IDX_HEADS)
IN_WIDTH = sum(SPLITS)
SPLIT_POINTS = tuple(int(v) for v in np.cumsum(SPLITS)[:-1])

kernel_name = "hymba_pool_dsa_sandwich_trunk"


def rms_norm(x, g):
    xf = x.astype(jnp.float32)
    y = xf * lax.rsqrt(jnp.mean(xf * xf, axis=-1, keepdims=True) + EPS)
    return (y * g.astype(jnp.float32)).astype(x.dtype)


def rope_partial(x, pos, rot_dim):
    half = rot_dim // 2
    inv = ROPE_THETA ** (-jnp.arange(half, dtype=jnp.float32) / half)
    ang = pos.astype(jnp.float32)[..., None] * inv
    cos = jnp.cos(ang)[:, :, None, :]
    sin = jnp.sin(ang)[:, :, None, :]
    x1 = x[..., :half].astype(jnp.float32)
    x2 = x[..., half:rot_dim].astype(jnp.float32)
    rot = jnp.concatenate([x1 * cos - x2 * sin, x2 * cos + x1 * sin], axis=-1)
    return jnp.concatenate([rot.astype(x.dtype), x[..., rot_dim:]], axis=-1)


def pool_mixer(u, w_pool, pool_scale):
    B, S, _ = u.shape
    ug = u.reshape(B, S, N_POOL_GROUPS, POOL_GROUP)
    cs = jnp.cumsum(ug.astype(jnp.float32), axis=1)
    t = jnp.arange(S)
    outs = []
    for g, w in enumerate(POOL_WINDOWS):
        c = cs[:, :, g]
        lagged = jnp.pad(c, ((0, 0), (w, 0), (0, 0)))[:, :S]
        cnt = jnp.minimum(t + 1, w).astype(jnp.float32)[None, :, None]
        outs.append((c - lagged) / cnt - ug[:, :, g].astype(jnp.float32))
    d = jnp.stack(outs, axis=2).astype(u.dtype)
    y = jnp.einsum('bsgc,gcd->bsgd', d, w_pool)
    return y.reshape(B, S, POOL_WIDTH) * pool_scale


def dsa_attention(q, k, v, iq, ik, iw, topk):
    B, S = q.shape[0], q.shape[1]
    n_blocks = S // Q_BLOCK
    key_chunk = jnp.arange(S) // CHUNK
    ik32 = ik.astype(jnp.float32)

    def block(i):
        start = i * Q_BLOCK
        qb = lax.dynamic_slice_in_dim(q, start, Q_BLOCK, axis=1)
        iqb = lax.dynamic_slice_in_dim(iq, start, Q_BLOCK, axis=1).astype(jnp.float32)
        iwb = lax.dynamic_slice_in_dim(iw, start, Q_BLOCK, axis=1).astype(jnp.float32)
        q_chunk = (start + jnp.arange(Q_BLOCK)) // CHUNK
        allowed = key_chunk[None, :] <= q_chunk[:, None]
        logits = jnp.einsum('bqhd,bsd->bqhs', iqb, ik32) * (IDX_DIM ** -0.5)
        score = jnp.einsum('bqh,bqhs->bqs', iwb, jax.nn.relu(logits))
        score = jnp.where(allowed[None], score, -jnp.inf)
        top_val, top_idx = lax.top_k(score, topk)
        valid = jnp.isfinite(top_val)
        kg = jax.vmap(lambda kk, idx: kk[idx])(k, top_idx)
        vg = jax.vmap(lambda vv, idx: vv[idx])(v, top_idx)
        s = jnp.einsum('bqhd,bqkhd->bqhk', qb.astype(jnp.float32), kg.astype(jnp.float32)) * (HEAD_DIM ** -0.5)
        s = jnp.where(valid[:, :, None, :], s, -jnp.inf)
        p = jax.nn.softmax(s, axis=-1)
        o = jnp.einsum('bqhk,bqkhd->bqhd', p, vg.astype(jnp.float32))
        return o.astype(q.dtype)

    out = lax.map(block, jnp.arange(n_blocks))
    return jnp.transpose(out, (1, 0, 2, 3, 4)).reshape(B, S, N_HEADS * HEAD_DIM)


def setup_inputs(seed: int = 0) -> dict:
    key = jax.random.key(seed)
    ks = jax.random.split(key, 12)
    f32 = jnp.float32
    def gain(k):
        return 1.0 + 0.02 * jax.random.normal(k, (DEPTH, D_MODEL), f32)
    x = jax.random.normal(ks[0], (BATCH, SEQ, D_MODEL), f32)
    positions = jnp.broadcast_to(jnp.arange(SEQ, dtype=jnp.int32)[None, :], (BATCH, SEQ)).astype(jnp.int32)
    return {
        "x": x,
        "positions": positions,
        "g_pre_mix": gain(ks[1]),
        "w_in": jax.random.normal(ks[2], (DEPTH, D_MODEL, IN_WIDTH), f32) * D_MODEL ** -0.5,
        "w_pool": jax.random.normal(ks[3], (DEPTH, N_POOL_GROUPS, POOL_GROUP, POOL_GROUP), f32) * POOL_GROUP ** -0.5,
        "pool_scale": 1.0 + 0.02 * jax.random.normal(ks[4], (DEPTH, POOL_WIDTH), f32),
        "w_out": jax.random.normal(ks[5], (DEPTH, D_MODEL, D_MODEL), f32) * D_MODEL ** -0.5,
        "g_post_mix": gain(ks[6]),
        "g_pre_ffn": gain(ks[7]),
        "w_ff1": jax.random.normal(ks[8], (DEPTH, D_MODEL, D_FF), f32) * D_MODEL ** -0.5,
        "w_ff2": jax.random.normal(ks[9], (DEPTH, D_FF, D_MODEL), f32) * D_FF ** -0.5,
        "g_post_ffn": gain(ks[10]),
    }


def reference(x, positions, g_pre_mix, w_in, w_pool, pool_scale, w_out, g_post_mix,
              g_pre_ffn, w_ff1, w_ff2, g_post_ffn):
    B, S, _ = x.shape
    topk = min(TOPK_MAX, S // 4)
    for l in range(DEPTH):
        h = rms_norm(x, g_pre_mix[l])
        proj = h @ w_in[l]
        u_pool, q, k, v, iq, ik, iw = jnp.split(proj, SPLIT_POINTS, axis=-1)
        q = rope_partial(q.reshape(B, S, N_HEADS, HEAD_DIM), positions, ROPE_DIM)
        k = rope_partial(k.reshape(B, S, N_HEADS, HEAD_DIM), positions, ROPE_DIM)
        v = v.reshape(B, S, N_HEADS, HEAD_DIM)
        iq = rope_partial(iq.reshape(B, S, IDX_HEADS, IDX_DIM), positions, IDX_ROPE_DIM)
        ik = rope_partial(ik[:, :, None, :], positions, IDX_ROPE_DIM)[:, :, 0, :]
        iw = iw * (IDX_HEADS ** -0.5)
        a_out = pool_mixer(u_pool, w_pool[l], pool_scale[l])
        b_out = dsa_attention(q, k, v, iq, ik, iw, topk)
        mix = jnp.concatenate([a_out, b_out], axis=-1) @ w_out[l]
        x = x + rms_norm(mix, g_post_mix[l])
        h = rms_norm(x, g_pre_ffn[l])
        f = jnp.square(jax.nn.relu(h @ w_ff1[l])) @ w_ff2[l]
        x = x + rms_norm(f, g_post_ffn[l])
    return x
```

```python
import numpy as np
import ml_dtypes
import concourse.bass as bass
import concourse.mybir as mybir
from concourse.ap import AP
from concourse.bass_utils import run_bass_kernel_spmd

F32 = mybir.dt.float32
BF16 = mybir.dt.bfloat16
I32 = mybir.dt.int32
ALU = mybir.AluOpType
AF = mybir.ActivationFunctionType
AX = mybir.AxisListType

D = 1024
S = 4096
DEPTH = 4
INW = 2632
DFF = 4096
NT = S // 128
EPS = 1e-6
NIT = 16
TOPK = 256
NEG = -1.0e30
CH_C = 256
TWO_PI = 6.283185307179586
PI_LO = 3.1415925

ENGS = ("pe", "act", "dve", "pool", "sp")


def C(method, *args, **kwargs):
    return (method, args, kwargs)
NSLOT = 8


class Op:
    __slots__ = ("eng", "fn", "deps", "sig", "sigval", "dma", "slot", "prev", "done", "idx", "nd")

    def __init__(self, eng, fn, dma, nd):
        self.eng = eng
        self.fn = fn
        self.dma = dma
        self.nd = nd
        self.deps = []
        self.sig = False
        self.sigval = 0
        self.slot = 0
        self.prev = 0
        self.done = 0
        self.idx = 0


class Sched:
    def __init__(self):
        self.ops = {e: [] for e in ENGS}
        self.last_w = {}
        self.readers = {}
        self.dma_live = []

    def add(self, eng, fn, reads=(), writes=(), dma=False, nd=1):
        op = Op(eng, fn, dma, nd)
        op.idx = len(self.ops[eng])
        deps = {}

        def need(d, kind):
            if d is None:
                return
            if d.dma:
                deps[("d", id(d))] = d
                return
            if d.eng == eng and not dma:
                if eng == "pe" or kind == "war":
                    return
            k = ("e", d.eng)
            if k not in deps or deps[k].idx < d.idx:
                deps[k] = d

        for r in reads:
            need(self.last_w.get(r), "raw")
        for w in writes:
            need(self.last_w.get(w), "waw")
            rd = self.readers.get(w)
            if rd:
                for d in rd.values():
                    need(d, "war")
        op.deps = list(deps.values())
        for w in writes:
            self.last_w[w] = op
            self.readers[w] = {}
        for r in reads:
            key = ("d", id(op)) if dma else ("e", eng)
            self.readers.setdefault(r, {})[key] = op
        self.ops[eng].append(op)
        if dma:
            self.dma_live.append(op)
        return op

    def barrier(self):
        lasts = []
        for e in ENGS:
            for op in reversed(self.ops[e]):
                if not op.dma and op.fn is not None:
                    lasts.append(op)
                    break
        dmas = list(self.dma_live)
        for e in ENGS:
            op = Op(e, None, False, 0)
            op.idx = len(self.ops[e])
            op.deps = list(lasts) + dmas
            self.ops[e].append(op)
        self.last_w.clear()
        self.readers.clear()
        self.dma_live = []

    def emit(self, nc):
        from contextlib import ExitStack
        for e in ENGS:
            for op in self.ops[e]:
                for d in op.deps:
                    d.sig = True
        for e in ENGS:
            cnt = 0
            ndma = 0
            tot = [0] * NSLOT
            for op in self.ops[e]:
                if op.dma:
                    op.slot = ndma % NSLOT
                    ndma += 1
                    op.prev = tot[op.slot]
                    tot[op.slot] += 16 * op.nd
                    op.done = tot[op.slot]
                elif op.sig:
                    cnt += 1
                    op.sigval = cnt
        with ExitStack() as es:
            sems = {e: es.enter_context(nc.semaphore("s_" + e)) for e in ENGS}
            dsems = {}
            for e in ENGS:
                if any(op.dma for op in self.ops[e]):
                    for i in range(NSLOT):
                        dsems[(e, i)] = es.enter_context(nc.semaphore("d_%s%d" % (e, i)))
            block = es.enter_context(nc.Block())

            def run(e, eng):
                waited = {}
                for op in self.ops[e]:
                    for d in op.deps:
                        if d.dma:
                            key = ("d", d.eng, d.slot)
                            val = d.done
                            sem = dsems[(d.eng, d.slot)]
                        else:
                            key = ("e", d.eng)
                            val = d.sigval
                            sem = sems[d.eng]
                        if waited.get(key, 0) < val:
                            eng.wait_ge(sem, val)
                            waited[key] = val
                    if op.dma:
                        key = ("d", e, op.slot)
                        sem = dsems[(e, op.slot)]
                        if waited.get(key, 0) < op.prev:
                            eng.wait_ge(sem, op.prev)
                            waited[key] = op.prev
                        ins = [getattr(eng, m_)(*a_, **k_) for (m_, a_, k_) in op.fn]
                        assert len(ins) == op.nd
                        for i_ in ins:
                            i_.then_inc(sem, 16)
                    elif op.fn is not None:
                        m_, a_, k_ = op.fn
                        i_ = getattr(eng, m_)(*a_, **k_)
                        if op.sig:
                            i_.then_inc(sems[e], 1)

            @block.tensor
            def _(t):
                run("pe", t)

            @block.scalar
            def _(t):
                run("act", t)

            @block.vector
            def _(t):
                run("dve", t)

            @block.gpsimd
            def _(t):
                run("pool", t)

            @block.sync
            def _(t):
                run("sp", t)


class Arena:
    def __init__(self, nc, base, limit):
        self.nc = nc
        self.base = base
        self.off = base
        self.limit = limit
        self.n = 0

    def t(self, name, shape, dtype):
        esz = 4 if dtype in (F32, I32) else 2
        nbytes = esz
        for s_ in shape[1:]:
            nbytes *= s_
        nbytes = (nbytes + 31) // 32 * 32
        off = self.off
        self.off += nbytes
        assert self.off <= self.limit, (name, self.off, self.limit)
        self.n += 1
        return self.nc.alloc_sbuf_tensor_at("%s_%d_%d" % (name, off, self.n), list(shape), dtype, offset=off)

    def mark(self):
        return self.off

    def reset(self, off):
        self.off = off


def row_elems(t):
    n = 1
    for s_ in t.shape[1:]:
        n *= s_
    return n


def build_program(n_layers=DEPTH, debug=False, stop_after=None, n_tiles=NT, sub=99, max_ci=99, qsteps=99):
    nc = bass.Bass("TRN2", target_bir_lowering=False)
    sc = Sched()
    add = sc.add

    def din(name, shape, dt=F32):
        return nc.dram_tensor(name, list(shape), dt, kind="ExternalInput").ap()

    x_in = din("x", [S, D])
    pos_in = din("pos", [128, NT], I32)
    g_pre_mix = din("g_pre_mix", [n_layers, D])
    w_in = din("w_in", [n_layers, D, INW])
    w_pool = din("w_pool", [n_layers, 4, 128, 128])
    pscale_in = din("pool_scale_t", [n_layers, 128, 4])
    w_out = din("w_out", [n_layers, D, D])
    g_post_mix = din("g_post_mix", [n_layers, D])
    g_pre_ffn = din("g_pre_ffn", [n_layers, D])
    w_ff1 = din("w_ff1", [n_layers, D, DFF])
    w_ff2 = din("w_ff2", [n_layers, DFF, D])
    g_post_ffn = din("g_post_ffn", [n_layers, D])
    ident_in = din("c_ident", [128, 128], BF16)
    bands_in = din("c_bands", [128, 12, 128], BF16)
    invf_in = din("c_invf", [128, 8])
    y_out = nc.dram_tensor("y", [S, D], F32, kind="ExternalOutput").ap()

    skind = "ExternalOutput" if debug else "Internal"
    x_mid = nc.dram_tensor("x_mid", [S, D], F32, kind=skind).ap()
    x_nxt = nc.dram_tensor("x_nxt", [S, D], F32).ap()
    qT_s = nc.dram_tensor("qT_s", [4, 128, S], BF16, kind=skind).ap()
    aT_s = nc.dram_tensor("aT_s", [4, 128, S], BF16, kind=skind).ap()
    mT_s = nc.dram_tensor("mT_s", [NT, 128, S], BF16, kind=skind).ap()

    PS = [nc.alloc_psum_tensor("bank%d" % i, [128, 512], F32) for i in range(8)]

    def psb(i):
        return PS[i][:, :].bitcast(BF16)

    total = nc.sbuf_bytes_remaining
    guard = nc.alloc_sbuf_tensor("arena", [128, (total - 64) // 4], F32)
    base0 = int(nc.lookup_mloc(guard).addr)
    ar = Arena(nc, base0, base0 + (total - 64) // 4 * 4)

    ident = ar.t("ident", [128, 128], BF16)
    bands = ar.t("bands", [128, 12, 128], BF16)
    invf = ar.t("invf", [128, 8], F32)
    posi = ar.t("posi", [128, NT], I32)
    posf = ar.t("posf", [128, NT], F32)
    cos2 = ar.t("cos2", [128, NT, 16], F32)
    sinm = ar.t("sinm", [128, NT, 16], F32)
    pw2 = ar.t("pw2", [128, NIT + 2], F32)
    onesb = ar.t("onesb", [128, 64], BF16)
    negbig = ar.t("negbig", [128, 1], F32)
    small_mark = ar.mark()
    kT = ar.t("kT", [128, 4, S], BF16)
    Vext = ar.t("Vext", [128, NT, 520], BF16)
    phase_mark = ar.mark()

    add("sp", [C("dma_start", out=ident[:, :], in_=ident_in)], writes=["ident"], dma=True)
    add("sp", [C("dma_start", out=bands[:, :, :], in_=bands_in)], writes=["bands"], dma=True)
    add("sp", [C("dma_start", out=invf[:, :], in_=invf_in)], writes=["invf"], dma=True)
    add("sp", [C("dma_start", out=posi[:, :], in_=pos_in)], writes=["posi"], dma=True)
    for k in range(NIT + 2):
        add("pool", C("memset", pw2[:, k:k + 1], float(2.0 ** (-k))), writes=["pw2"])
    add("pool", C("memset", onesb[:, :], 1.0), writes=["onesb"])
    add("pool", C("memset", negbig[:, :], NEG), writes=["negbig"])
    m0 = ar.mark()
    ang = ar.t("ang", [128, NT, 8], F32)
    a1 = ar.t("a1", [128, NT, 8], F32)
    ki = ar.t("ki", [128, NT, 8], I32)
    kf = ar.t("kf", [128, NT, 8], F32)
    rr = ar.t("rr", [128, NT, 8], F32)
    mm = ar.t("mm", [128, NT, 8], F32)
    sn = ar.t("sn", [128, NT, 8], F32)
    add("dve", C("tensor_copy", out=posf[:, :], in_=posi[:, :]), reads=["posi"], writes=["posf"])
    posb = AP(posf, 0, [[NT, 128], [1, NT], [0, 8]])
    invb = AP(invf, 0, [[8, 128], [0, NT], [1, 8]])
    add("dve", C("tensor_tensor", out=ang[:, :, :], in0=posb, in1=invb, op=ALU.mult),
        reads=["posf", "invf"], writes=["ang"])
    for which in range(2):
        shift = np.pi if which == 0 else (np.pi + np.pi / 2)
        add("dve", C("tensor_scalar", out=a1[:, :, :], in0=ang[:, :, :], scalar1=float(shift),
                                                         scalar2=None, op0=ALU.add),
            reads=["ang"], writes=["a1"])
        add("dve", C("tensor_scalar", out=kf[:, :, :], in0=a1[:, :, :], scalar1=float(1.0 / TWO_PI),
                                             scalar2=None, op0=ALU.mult), reads=["a1"], writes=["kf"])
        add("dve", C("tensor_copy", out=ki[:, :, :], in_=kf[:, :, :]), reads=["kf"], writes=["ki"])
        add("dve", C("tensor_copy", out=kf[:, :, :], in_=ki[:, :, :]), reads=["ki"], writes=["kf"])
        add("dve", C("scalar_tensor_tensor", out=rr[:, :, :], in0=kf[:, :, :], scalar=float(-TWO_PI),
                                                    in1=a1[:, :, :], op0=ALU.mult, op1=ALU.add),
            reads=["kf", "a1"], writes=["rr"])
        add("dve", C("tensor_scalar", out=rr[:, :, :], in0=rr[:, :, :], scalar1=float(-np.pi),
                                             scalar2=None, op0=ALU.add), reads=["rr"], writes=["rr"])
        add("dve", C("tensor_scalar", out=mm[:, :, :], in0=rr[:, :, :], scalar1=float(-np.pi),
                                             scalar2=float(TWO_PI), op0=ALU.is_lt, op1=ALU.mult),
            reads=["rr"], writes=["mm"])
        add("dve", C("tensor_tensor", out=rr[:, :, :], in0=rr[:, :, :], in1=mm[:, :, :], op=ALU.add),
            reads=["rr", "mm"], writes=["rr"])
        add("dve", C("tensor_scalar", out=rr[:, :, :], in0=rr[:, :, :], scalar1=float(PI_LO),
                                             scalar2=float(-PI_LO), op0=ALU.min, op1=ALU.max),
            reads=["rr"], writes=["rr"])
        add("act", C("activation", out=sn[:, :, :], in_=rr[:, :, :], func=AF.Sin), reads=["rr"], writes=["sn"])
        if which == 0:
            add("dve", C("tensor_scalar", out=sinm[:, :, 0:8], in0=sn[:, :, :], scalar1=-1.0, scalar2=None,
                                                 op0=ALU.mult), reads=["sn"], writes=["sinm"])
            add("dve", C("tensor_copy", out=sinm[:, :, 8:16], in_=sn[:, :, :]), reads=["sn"], writes=["sinm"])
        else:
            add("dve", C("tensor_copy", out=cos2[:, :, 0:8], in_=sn[:, :, :]), reads=["sn"], writes=["cos2"])
            add("dve", C("tensor_copy", out=cos2[:, :, 8:16], in_=sn[:, :, :]), reads=["sn"], writes=["cos2"])
    sc.barrier()
    ar.reset(m0)
    if stop_after == "init":
        sc.emit(nc)
        return nc

    def rstd_from_ss(ss_ap, lnv_ap, rstd_ap, rname, sname):
        add("act", C("activation", out=lnv_ap, in_=ss_ap, func=AF.Ln, bias=float(EPS), scale=float(1.0 / D)),
            reads=[sname], writes=[rname + "_ln"])
        add("act", C("activation", out=rstd_ap, in_=lnv_ap, func=AF.Exp, scale=-0.5),
            reads=[rname + "_ln"], writes=[rname])

    conv_rr = [0]

    def convert(out_ap, in_ap, reads, writes):
        engs = ("pool", "dve", "act")
        en = engs[conv_rr[0] % 3]
        conv_rr[0] += 1
        if en == "act":
            add("act", C("activation", out=out_ap, in_=in_ap, func=AF.Copy), reads=reads, writes=writes)
        else:
            add(en, C("tensor_copy", out=out_ap, in_=in_ap), reads=reads, writes=writes)

    for l in range(n_layers):
        x_cur = x_in if l == 0 else x_nxt
        x_fin = y_out if l == n_layers - 1 else x_nxt

        ar.reset(phase_mark)
        winb = ar.t("winb", [128, 8, INW], BF16)
        wpb = ar.t("wpb", [128, 4, 128], BF16)
        ikT = ar.t("ikT", [128, S], BF16)
        a_work = ar.mark()
        add("pool", C("memset", Vext[:, :, :], 1.0), writes=["Vext_all"])
        stg = [ar.t("stgA%d" % i, [128, INW], F32) for i in range(2)]
        for k in range(8):
            b = k % 2
            add("sp", [C("dma_start", out=stg[b][:, :], in_=w_in[l, k * 128:(k + 1) * 128, :])],
                writes=[("stgA", b)], dma=True)
            h0 = 1280
            convert(winb[:, k, 0:h0], stg[b][:, 0:h0], [("stgA", b)], [("winb", k, 0)])
            convert(winb[:, k, h0:INW], stg[b][:, h0:INW], [("stgA", b)], [("winb", k, 1)])
        for g in range(4):
            b = g % 2
            add("sp", [C("dma_start", out=stg[b][:, 0:128], in_=w_pool[l, g, :, :])],
                writes=[("stgA", b)], dma=True)
            convert(wpb[:, g, :], stg[b][:, 0:128], [("stgA", b)], [("wpb", g)])
        sc.barrier()
        ar.reset(a_work)
        if stop_after == "A0":
            sc.emit(nc)
            return nc

        gB = ar.t("gBA", [128, D], F32)
        pscale = ar.t("pscale", [128, 4], F32)
        xt = [ar.t("xtA%d" % i, [128, D], F32) for i in range(2)]
        sqj = ar.t("sqjA", [128, D], BF16)
        hn = ar.t("hnA", [128, D], F32)
        hb = ar.t("hbA", [128, D], BF16)
        hT = [ar.t("hTA%d" % i, [128, D], BF16) for i in range(2)]
        utok = [ar.t("utok%d" % i, [128, 512], BF16) for i in range(2)]
        qtok = ar.t("qtok", [128, 512], BF16)
        ktok = ar.t("ktok", [128, 512], BF16)
        iqtok = ar.t("iqtok", [128, 512], BF16)
        iktok = ar.t("iktok", [128, 128], BF16)
        qTt = [ar.t("qTt%d" % i, [128, 4, 128], BF16) for i in range(2)]
        iqTt = [ar.t("iqTt%d" % i, [128, 4, 128], BF16) for i in range(2)]
        dTb = ar.t("dTb", [128, 512], BF16)
        aTt = [ar.t("aTt%d" % i, [128, 4, 128], BF16) for i in range(2)]
        mTt = [ar.t("mTt%d" % i, [128, 8, 128], BF16) for i in range(2)]
        ropeA = ar.t("ropeA", [128, 8, 16], F32)
        ropeB = ar.t("ropeB", [128, 8, 16], F32)
        iwt = [ar.t("iwt%d" % i, [128, 8], F32) for i in range(2)]
        Dh = [ar.t("Dh%d" % i, [128, 8, 128], BF16) for i in range(2)]
        Rh = [ar.t("Rh%d" % i, [128, 512], BF16) for i in range(4)]
        scb = ar.t("scb", [128, S], F32)
        mk = ar.t("mk", [128, S], BF16)
        sm = ar.t("smA", [128, 64], F32)
        hk = ar.t("hk", [128, NIT + 2], F32)
        stq = [ar.t("stq%d" % i, [128, 512], F32) for i in range(2)]
        stq_rr = [0]
        stw = ar.t("stw", [128, 72], F32)

        add("sp", [C("dma_start", out=gB[:, :], in_=g_pre_mix[l:l + 1, :].to_broadcast([128, D]))],
            writes=["gBA"], dma=True)
        add("sp", [C("dma_start", out=pscale[:, :], in_=pscale_in[l, :, :])], writes=["pscale"], dma=True)

        def loadx(tt):
            b = tt % 2
            add("sp", [C("dma_start", out=xt[b][:, :], in_=x_cur[tt * 128:(tt + 1) * 128, :])],
                writes=[("xtA", b)], dma=True)

        def rope(src_ps, dst, nh, tt, rname, wname, qs=99):
            si = stq_rr[0] % 2
            stq_rr[0] += 1
            wcols = nh * 64
            add("act", C("activation", out=stq[si][:, 0:wcols], in_=src_ps, func=AF.Copy),
                reads=[rname], writes=[("stq", si)])
            sv = stq[si][:, 0:wcols].rearrange("p (h d) -> p h d", h=nh)
            dv = dst.rearrange("p (h d) -> p h d", h=nh)
            cb = AP(cos2, tt * 16, [[NT * 16, 128], [0, nh], [1, 16]])
            s1 = AP(sinm, tt * 16, [[NT * 16, 128], [0, nh], [1, 8]])
            s2 = AP(sinm, tt * 16 + 8, [[NT * 16, 128], [0, nh], [1, 8]])
            add("pool", C("tensor_copy", out=dv[:, :, 16:64], in_=sv[:, :, 16:64]),
                reads=[("stq", si)], writes=[wname + "_r"])
            add("dve", C("tensor_tensor", out=ropeA[:, 0:nh, :], in0=sv[:, :, 0:16], in1=cb, op=ALU.mult),
                reads=[("stq", si), "cos2"], writes=["ropeA"])
            add("dve", C("tensor_tensor", out=ropeB[:, 0:nh, 0:8], in0=sv[:, :, 8:16], in1=s1, op=ALU.mult),
                reads=[("stq", si), "sinm"], writes=["ropeB0"])
            add("dve", C("tensor_tensor", out=ropeB[:, 0:nh, 8:16], in0=sv[:, :, 0:8], in1=s2, op=ALU.mult),
                reads=[("stq", si), "sinm"], writes=["ropeB1"])
            add("dve", C("tensor_tensor", out=dv[:, :, 0:16], in0=ropeA[:, 0:nh, :], in1=ropeB[:, 0:nh, :],
                                                 op=ALU.add),
                reads=["ropeA", "ropeB0", "ropeB1"], writes=[wname + "_h"])
            return si

        def rope_sb(src_sb, dst, nh, tt, rname, wname):
            sv = src_sb.rearrange("p (h d) -> p h d", h=nh)
            dv = dst.rearrange("p (h d) -> p h d", h=nh)
            cb = AP(cos2, tt * 16, [[NT * 16, 128], [0, nh], [1, 16]])
            s1 = AP(sinm, tt * 16, [[NT * 16, 128], [0, nh], [1, 8]])
            s2 = AP(sinm, tt * 16 + 8, [[NT * 16, 128], [0, nh], [1, 8]])
            add("pool", C("tensor_copy", out=dv[:, :, 16:64], in_=sv[:, :, 16:64]),
                reads=[rname], writes=[wname + "_r"])
            add("dve", C("tensor_tensor", out=ropeA[:, 0:nh, :], in0=sv[:, :, 0:16], in1=cb, op=ALU.mult),
                reads=[rname, "cos2"], writes=["ropeA"])
            add("dve", C("tensor_tensor", out=ropeB[:, 0:nh, 0:8], in0=sv[:, :, 8:16], in1=s1, op=ALU.mult),
                reads=[rname, "sinm"], writes=["ropeB0"])
            add("dve", C("tensor_tensor", out=ropeB[:, 0:nh, 8:16], in0=sv[:, :, 0:8], in1=s2, op=ALU.mult),
                reads=[rname, "sinm"], writes=["ropeB1"])
            add("dve", C("tensor_tensor", out=dv[:, :, 0:16], in0=ropeA[:, 0:nh, :], in1=ropeB[:, 0:nh, :],
                                                 op=ALU.add),
                reads=["ropeA", "ropeB0", "ropeB1"], writes=[wname + "_h"])

        CH = [(0, 512), (512, 512), (1024, 512), (1536, 512), (2048, 512), (2560, 72)]

        def stageX(tt):
            b = tt % 2
            xtb = xt[b]
            add("act", C("activation", out=sqj[:, :], in_=xtb[:, :], func=AF.Square, accum_out=sm[:, 0:1]),
                reads=[("xtA", b)], writes=["sqjA", "ssA"])
            rstd_from_ss(sm[:, 0:1], sm[:, 1:2], sm[:, 2:3], "rstdA", "ssA")
            add("pool", C("tensor_scalar", out=hn[:, :], in0=xtb[:, :], scalar1=sm[:, 2:3], scalar2=None,
                                                  op0=ALU.mult),
                reads=[("xtA", b), "rstdA"], writes=["hnA"])
            add("pool", C("tensor_tensor", out=hb[:, :], in0=hn[:, :], in1=gB[:, :], op=ALU.mult),
                reads=["hnA", "gBA"], writes=["hbA"])
            if sub < 1:
                return
            pT = psb(3)
            for k in range(8):
                add("pe", C("transpose", pT[:, k * 128:(k + 1) * 128], hb[:, k * 128:(k + 1) * 128],
                                                     ident[:, :]),
                    reads=["hbA", "ident"], writes=[("bank", 3)])
            add("act", C("activation", out=hT[b][:, :], in_=pT[:, :], func=AF.Copy),
                reads=[("bank", 3)], writes=[("hTA", b)])
            if sub < 2:
                return
            for ci, (c0, wc) in enumerate(CH):
                if ci > max_ci:
                    break
                bk = ci % 3
                pj = PS[bk]
                for k in range(8):
                    add("pe", C("matmul",
                        pj[:, 0:wc], hT[b][:, k * 128:(k + 1) * 128], winb[:, k, c0:c0 + wc],
                        start=(k == 0), stop=(k == 7)),
                        reads=[("hTA", b)], writes=[("bank", bk)])
                rn = ("bank", bk)
                if ci == 0:
                    add("act", C("activation", out=utok[b][:, :], in_=pj[:, :], func=AF.Copy),
                        reads=[rn], writes=[("utok", b)])
                elif ci == 1:
                    rope(pj[:, 0:512], qtok[:, :], 8, tt, rn, "qtok")
                    pq = psb(4)
                    for c in range(4):
                        add("pe", C("transpose", pq[:, c * 128:(c + 1) * 128],
                                                             qtok[:, c * 128:(c + 1) * 128], ident[:, :]),
                            reads=["qtok_r", "qtok_h", "ident"], writes=[("bank", 4, 0)])
                    add("act", C("activation", out=qTt[b][:, :, :].rearrange("p c t -> p (c t)"),
                                                      in_=pq[:, 0:512], func=AF.Copy),
                        reads=[("bank", 4, 0)], writes=[("qTt", b)])
                    add("sp", [C("dma_start",
                        out=qT_s[:, :, tt * 128:(tt + 1) * 128].rearrange("c p t -> p c t"), in_=qTt[b][:, :, :])],
                        reads=[("qTt", b)], writes=[("qT_s", tt)], dma=True)
                elif ci == 2:
                    rope(pj[:, 0:512], ktok[:, :], 8, tt, rn, "ktok")
                    pq = psb(4)
                    for c in range(4):
                        add("pe", C("transpose", pq[:, 512 + c * 128:512 + (c + 1) * 128],
                                                             ktok[:, c * 128:(c + 1) * 128], ident[:, :]),
                            reads=["ktok_r", "ktok_h", "ident"], writes=[("bank", 4, 1)])
                    add("dve", C("tensor_copy",
                        out=kT[:, :, tt * 128:(tt + 1) * 128],
                        in_=pq[:, 512:1024].rearrange("p (c t) -> p c t", c=4)),
                        reads=[("bank", 4, 1)], writes=[("kT", tt)])
                elif ci == 3:
                    add("act", C("activation",
                        out=AP(Vext, tt * 520, [[NT * 520, 128], [65, 8], [1, 64]]),
                        in_=pj[:, 0:512].rearrange("p (h d) -> p h d", h=8), func=AF.Copy),
                        reads=[rn], writes=[("Vext", tt)])
                elif ci == 4:
                    rope(pj[:, 0:512], iqtok[:, :], 8, tt, rn, "iqtok")
                    pq = psb(3)
                    for c in range(4):
                        add("pe", C("transpose", pq[:, c * 128:(c + 1) * 128],
                                                             iqtok[:, c * 128:(c + 1) * 128], ident[:, :]),
                            reads=["iqtok_r", "iqtok_h", "ident"], writes=[("bank", 3)])
                    add("act", C("activation", out=iqTt[b][:, :, :].rearrange("p c t -> p (c t)"),
                                                      in_=pq[:, 0:512], func=AF.Copy),
                        reads=[("bank", 3)], writes=[("iqTt", b)])
                else:
                    add("act", C("activation", out=stw[:, 0:72], in_=pj[:, 0:72], func=AF.Copy),
                        reads=[rn], writes=["stw"])
                    rope_sb(stw[:, 0:64], iktok[:, 0:64], 1, tt, "stw", "iktok")
                    add("pool", C("tensor_copy", out=iktok[:, 64:128], in_=iktok[:, 0:64]),
                        reads=["iktok_r", "iktok_h"], writes=["iktok_d"])
                    add("dve", C("tensor_scalar", out=iwt[b][:, :], in0=stw[:, 64:72],
                                                                scalar1=float(8 ** -0.5 * 64 ** -0.5), scalar2=None,
                                                                op0=ALU.mult),
                        reads=["stw"], writes=[("iwt", b)])
                    pq = psb(3)
                    add("pe", C("transpose", pq[:, 512:640], iktok[:, :], ident[:, :]),
                        reads=["iktok_r", "iktok_h", "iktok_d", "ident"], writes=[("bank", 3)])
                    add("act", C("activation", out=ikT[:, tt * 128:(tt + 1) * 128], in_=pq[:, 512:640],
                                                      func=AF.Copy),
                        reads=[("bank", 3)], writes=[("ikT", tt)])
                    for h in range(8):
                        add("dve", C("tensor_scalar", out=Dh[b][:, h, :], in0=ident[:, :],
                                                                  scalar1=iwt[b][:, h:h + 1], scalar2=None,
                                                                  op0=ALU.mult),
                            reads=[("iwt", b), "ident"], writes=[("Dh", b, h)])
            if sub < 3:
                return
            pd = PS[5]
            for g in range(4):
                first = (tt == 0)
                add("pe", C("matmul",
                    pd[:, g * 128:(g + 1) * 128], utok[b][:, g * 128:(g + 1) * 128],
                    bands[:, 3 * g + (2 if first else 0), :], start=True, stop=first),
                    reads=[("utok", b), "bands"], writes=[("bank", 5)])
                if not first:
                    add("pe", C("matmul",
                        pd[:, g * 128:(g + 1) * 128], utok[1 - b][:, g * 128:(g + 1) * 128],
                        bands[:, 3 * g + 1, :], start=False, stop=True),
                        reads=[("utok", 1 - b), "bands"], writes=[("bank", 5)])
            add("act", C("activation", out=dTb[:, :], in_=pd[:, :], func=AF.Copy),
                reads=[("bank", 5)], writes=["dTb"])
            for g in range(4):
                add("pe", C("matmul", pd[:, g * 128:(g + 1) * 128], wpb[:, g, :],
                                                  dTb[:, g * 128:(g + 1) * 128], start=True, stop=True),
                    reads=["dTb"], writes=[("bank", 5)])
            for g in range(4):
                add("dve", C("tensor_scalar", out=aTt[b][:, g, :], in0=pd[:, g * 128:(g + 1) * 128],
                                                          scalar1=pscale[:, g:g + 1], scalar2=None, op0=ALU.mult),
                    reads=[("bank", 5), "pscale"], writes=[("aTt", b)])
            add("sp", [C("dma_start", out=aT_s[:, :, tt * 128:(tt + 1) * 128].rearrange("g p t -> p g t"),
                                               in_=aTt[b][:, :, :])],
                reads=[("aTt", b)], writes=[("aT_s", tt)], dma=True)
            if sub < 4:
                return
            NA = 128 * (tt + 1)
            nkc = (NA + 511) // 512
            items = [(kc, h) for kc in range(nkc) for h in range(8)]

            def width(kc):
                return min(512, NA - 512 * kc)

            def Lmm(i):
                kc, h = items[i]
                n = width(kc)
                r0 = 64 * (h % 2)
                pr = h // 2
                bk = 6 + (i % 2)
                kts = list(range(kc * 4, kc * 4 + (n + 127) // 128))
                add("pe", C("matmul", PS[bk][:, 0:n], iqTt[b][r0:r0 + 64, pr, :],
                                             ikT[r0:r0 + 64, kc * 512:kc * 512 + n], start=True, stop=True),
                    reads=[("iqTt", b)] + [("ikT", t_) for t_ in kts], writes=[("bank", bk)])

            def Rev(i):
                kc, h = items[i]
                n = width(kc)
                bk = 6 + (i % 2)
                rb = i % 4
                if i % 2 == 0:
                    add("act", C("activation", out=Rh[rb][:, 0:n], in_=PS[bk][:, 0:n], func=AF.Relu),
                        reads=[("bank", bk)], writes=[("Rh", rb)])
                else:
                    add("dve", C("tensor_scalar", out=Rh[rb][:, 0:n], in0=PS[bk][:, 0:n], scalar1=0.0,
                                                         scalar2=None, op0=ALU.max),
                        reads=[("bank", bk)], writes=[("Rh", rb)])

            def Smm(i):
                kc, h = items[i]
                n = width(kc)
                rb = i % 4
                add("pe", C("matmul", PS[5][:, 0:n], Dh[b][:, h, :], Rh[rb][:, 0:n],
                                             start=(h == 0), stop=(h == 7)),
                    reads=[("Rh", rb), ("Dh", b, h)], writes=[("bank", 5)])
                if h == 7:
                    add("act", C("activation", out=scb[:, kc * 512:kc * 512 + n], in_=PS[5][:, 0:n],
                                                      func=AF.Copy),
                        reads=[("bank", 5)], writes=["scb"])

            Lmm(0)
            for i in range(len(items)):
                if i + 1 < len(items):
                    Lmm(i + 1)
                Rev(i)
                Smm(i)
            if sub < 5:
                return
            if tt >= 2:
                add("dve", C("tensor_reduce", out=sm[:, 8:9], in_=scb[:, 0:NA], axis=AX.X, op=ALU.max),
                    reads=["scb"], writes=["rmax"])
                add("dve", C("tensor_reduce", out=sm[:, 9:10], in_=scb[:, 0:NA], axis=AX.X, op=ALU.min),
                    reads=["scb"], writes=["rmin"])
            add("dve", C("memset", scb[0:64, NA - 64:NA], NEG), reads=["rmin", "rmax"] if tt >= 2 else [],
                writes=["scb"])
            if tt >= 2:
                add("dve", C("tensor_tensor", out=sm[:, 10:11], in0=sm[:, 8:9], in1=sm[:, 9:10],
                                                     op=ALU.subtract), reads=["rmax", "rmin"], writes=["w0"])
                add("dve", C("tensor_scalar", out=sm[:, 10:11], in0=sm[:, 10:11], scalar1=1.0001,
                                                     scalar2=1e-6, op0=ALU.mult, op1=ALU.add),
                    reads=["w0"], writes=["w0"])
                add("dve", C("tensor_scalar", out=hk[:, :], in0=pw2[:, :], scalar1=sm[:, 10:11], scalar2=None,
                                                     op0=ALU.mult), reads=["w0", "pw2"], writes=["hk"])
                add("dve", C("tensor_tensor", out=sm[:, 11:12], in0=sm[:, 9:10], in1=hk[:, 1:2], op=ALU.add),
                    reads=["rmin", "hk"], writes=["mid"])
                for k in range(1, NIT + 1):
                    kn = k + 1 if k < NIT else k
                    add("dve", C("tensor_scalar", out=mk[:, 0:NA], in0=scb[:, 0:NA],
                                                              scalar1=sm[:, 11:12], scalar2=None, op0=ALU.is_ge,
                                                              op1=ALU.add, accum_out=sm[:, 12:13]),
                        reads=["scb", "mid"], writes=["mk", "cnt"])
                    add("dve", C("tensor_tensor", out=sm[:, 13:14], in0=sm[:, 11:12],
                                                                in1=hk[:, kn:kn + 1], op=ALU.subtract),
                        reads=["mid", "hk"], writes=["tA"])
                    add("dve", C("tensor_scalar", out=sm[:, 14:15], in0=sm[:, 12:13],
                                                              scalar1=float(TOPK) - 0.5, scalar2=hk[:, k:k + 1],
                                                              op0=ALU.is_ge, op1=ALU.mult),
                        reads=["cnt", "hk"], writes=["mhk"])
                    add("dve", C("tensor_tensor", out=sm[:, 11:12], in0=sm[:, 13:14], in1=sm[:, 14:15],
                                                         op=ALU.add), reads=["tA", "mhk"], writes=["mid"])
                add("dve", C("tensor_scalar", out=mk[:, 0:NA], in0=scb[:, 0:NA], scalar1=sm[:, 11:12],
                                                     scalar2=None, op0=ALU.is_ge),
                    reads=["scb", "mid"], writes=["mk"])
            else:
                add("dve", C("tensor_scalar", out=mk[:, 0:NA], in0=scb[:, 0:NA], scalar1=-1.0e29,
                                                     scalar2=None, op0=ALU.is_ge),
                    reads=["scb"], writes=["mk"])

        def stageY(tt):
            nkt = tt + 1
            for g0 in range(0, nkt, 8):
                nb = min(8, nkt - g0)
                gi = (g0 // 8) % 2
                pm = psb(4)
                for j in range(nb):
                    add("pe", C("transpose", pm[:, j * 128:(j + 1) * 128],
                                                         mk[:, (g0 + j) * 128:(g0 + j + 1) * 128], ident[:, :]),
                        reads=["mk", "ident"], writes=[("bank", 4, 0), ("bank", 4, 1)])
                add("act", C("activation", out=mTt[gi][:, 0:nb, :].rearrange("p k q -> p (k q)"),
                                                  in_=pm[:, 0:nb * 128], func=AF.Copy),
                    reads=[("bank", 4, 0), ("bank", 4, 1)], writes=[("mTt", gi)])
                add("sp", [C("dma_start",
                    out=mT_s[g0:g0 + nb, :, tt * 128:(tt + 1) * 128].rearrange("k p q -> p k q"),
                    in_=mTt[gi][:, 0:nb, :])],
                    reads=[("mTt", gi)], writes=[("mT_s", tt, g0)], dma=True)

        loadx(0)
        for tt in range(n_tiles):
            if tt + 1 < n_tiles:
                loadx(tt + 1)
            stageX(tt)
            if sub >= 6:
                stageY(tt)
        sc.barrier()
        if stop_after == "A":
            sc.emit(nc)
            return nc

        ar.reset(phase_mark)
        woA = ar.t("woA", [128, 4, D], BF16)
        woB = ar.t("woB", [128, 8, D], BF16)
        gBp = ar.t("gBB", [128, D], F32)
        stgB = [ar.t("stgB%d" % i, [128, D], F32) for i in range(2)]
        mTc = ar.t("mTc", [128, NT, 512], BF16)
        qTc = ar.t("qTc", [128, 4, 512], BF16)
        aTc = ar.t("aTc", [128, 4, 512], BF16)
        Eb = [ar.t("Eb%d" % i, [128, 512], BF16) for i in range(3)]
        Pm = [ar.t("Pm%d" % i, [128, 512], BF16) for i in range(3)]
        boT = ar.t("boT", [128, 8, 512], BF16)
        rden = ar.t("rden", [128, 512], F32)
        rhi = ar.t("rhi", [128, 512], BF16)
        rlo = ar.t("rlo", [128, 512], BF16)
        bcs = ar.t("bcs", [128, 512], F32)
        xtB = [ar.t("xtB%d" % i, [128, D], F32) for i in range(2)]
        ytmp = ar.t("ytmp", [128, D], F32)
        xo = [ar.t("xoB%d" % i, [128, D], F32) for i in range(2)]
        sqB = ar.t("sqB", [128, 512], BF16)
        smB = ar.t("smB", [128, 16], F32)

        for g in range(4):
            b = g % 2
            add("sp", [C("dma_start", out=stgB[b][:, :], in_=w_out[l, g * 128:(g + 1) * 128, :])],
                writes=[("stgB", b)], dma=True)
            convert(woA[:, g, :], stgB[b][:, :], [("stgB", b)], [("woA", g)])
        for h in range(8):
            b = h % 2
            add("sp", [C("dma_start", out=stgB[b][0:64, :],
                                                       in_=w_out[l, 512 + 64 * h:512 + 64 * h + 64, :])],
                writes=[("stgB", b)], dma=True)
            convert(woB[0:64, h, :], stgB[b][0:64, :], [("stgB", b)], [("woB", h)])
        add("sp", [C("dma_start", out=gBp[:, :], in_=g_post_mix[l:l + 1, :].to_broadcast([128, D]))],
            writes=["gBB"], dma=True)

        for qc in range(8):
            KT = 4 * (qc + 1)
            add("sp", [C("dma_start", out=qTc[:, :, :],
                                                    in_=qT_s[:, :, qc * 512:(qc + 1) * 512].rearrange("c p t -> p c t"))],
                writes=["qTc"], dma=True)
            add("sp", [C("dma_start", out=aTc[:, :, :],
                                                    in_=aT_s[:, :, qc * 512:(qc + 1) * 512].rearrange("g p t -> p g t"))],
                writes=["aTc"], dma=True)
            for k0 in range(0, 4 * qc, 4):
                add("sp", [C("dma_start",
                    out=mTc[:, k0:k0 + 4, :],
                    in_=mT_s[k0:k0 + 4, :, qc * 512:(qc + 1) * 512].rearrange("k p q -> p k q"))],
                    writes=[("mTc", k0)], dma=True)
            for jb in range(4):
                ktb = 4 * qc + jb
                add("sp", [C("dma_start",
                    out=mTc[:, ktb, 128 * jb:512],
                    in_=mT_s[ktb, :, qc * 512 + 128 * jb:(qc + 1) * 512])],
                    writes=[("mTc", "b", ktb)], dma=True)
            cnt_i = [0]
            for h in range(8):
                r0 = 64 * (h % 2)
                pr = h // 2
                pob = 2 + (h % 2)
                po = PS[pob]

                def cstart(kt):
                    return 128 * max(0, kt - 4 * qc)

                def Smm(kt, i):
                    c0 = cstart(kt)
                    bk = i % 2
                    add("pe", C("matmul", PS[bk][:, c0:512], kT[r0:r0 + 64, pr, kt * 128:(kt + 1) * 128],
                                                 qTc[r0:r0 + 64, pr, c0:512], start=True, stop=True),
                        reads=[("kT", kt), "qTc"], writes=[("bank", bk)])

                def Ev(kt, i):
                    c0 = cstart(kt)
                    bk = i % 2
                    eb = i % 3
                    add("act", C("activation", out=Eb[eb][:, c0:512], in_=PS[bk][:, c0:512], func=AF.Exp,
                                                      scale=0.125),
                        reads=[("bank", bk)], writes=[("Eb", eb)])
                    en = "dve" if (i % 3) != 2 else "pool"
                    add(en, C("tensor_tensor", out=Pm[eb][:, c0:512], in0=Eb[eb][:, c0:512],
                                                      in1=mTc[:, kt, c0:512], op=ALU.mult),
                        reads=[("Eb", eb), (("mTc", (kt // 4) * 4) if kt < 4 * qc else ("mTc", "b", kt))], writes=[("Pm", eb)])

                def PV(kt, i):
                    c0 = cstart(kt)
                    eb = i % 3
                    add("pe", C("matmul", po[0:65, c0:512], Vext[:, kt, h * 65:h * 65 + 65],
                                                 Pm[eb][:, c0:512], start=(kt == 0), stop=(kt == KT - 1)),
                        reads=[("Pm", eb), ("Vext", kt)], writes=[("bank", pob)])

                base_i = cnt_i[0]
                Smm(0, base_i)
                for kt in range(KT):
                    if kt + 1 < KT:
                        Smm(kt + 1, base_i + kt + 1)
                    Ev(kt, base_i + kt)
                    PV(kt, base_i + kt)
                cnt_i[0] += KT
                add("dve", C("reciprocal", out=rden[64:65, :], in_=po[64:65, :]),
                    reads=[("bank", pob)], writes=["rden"])
                add("dve", C("tensor_copy", out=rhi[64:65, :], in_=rden[64:65, :]), reads=["rden"], writes=["rhi"])
                add("dve", C("tensor_tensor", out=rlo[64:65, :], in0=rden[64:65, :], in1=rhi[64:65, :],
                             op=ALU.subtract), reads=["rden", "rhi"], writes=["rlo"])
                add("pe", C("matmul", PS[4][0:64, :], onesb[64:65, 0:64], rhi[64:65, :], start=True, stop=False),
                    reads=["rhi", "onesb"], writes=[("bank", 4)])
                add("pe", C("matmul", PS[4][0:64, :], onesb[64:65, 0:64], rlo[64:65, :], start=False, stop=True),
                    reads=["rlo", "onesb"], writes=[("bank", 4)])
                add("act", C("activation", out=bcs[0:64, :], in_=PS[4][0:64, :], func=AF.Copy),
                    reads=[("bank", 4)], writes=["bcs"])
                add("dve", C("tensor_tensor", out=boT[0:64, h, :], in0=po[0:64, :],
                                                                 in1=bcs[0:64, :], op=ALU.mult),
                    reads=[("bank", pob), "bcs"], writes=[("boT", h)])
            for j in range(4):
                tt = qc * 4 + j
                xb = tt % 2
                add("sp", [C("dma_start", out=xtB[xb][:, :],
                                                               in_=x_cur[tt * 128:(tt + 1) * 128, :])],
                    writes=[("xtB", xb)], dma=True)
                for n in range(2):
                    pm_ = PS[5 + n]
                    for g in range(4):
                        add("pe", C("matmul",
                            pm_[:, :], aTc[:, g, j * 128:(j + 1) * 128], woA[:, g, n * 512:(n + 1) * 512],
                            start=(g == 0), stop=False),
                            reads=["aTc", ("woA", g)], writes=[("bank", 5 + n)])
                    for h in range(8):
                        add("pe", C("matmul",
                            pm_[:, :], boT[0:64, h, j * 128:(j + 1) * 128], woB[0:64, h, n * 512:(n + 1) * 512],
                            start=False, stop=(h == 7)),
                            reads=[("boT", h), ("woB", h)], writes=[("bank", 5 + n)])
                    add("act", C("activation", out=sqB[:, :], in_=pm_[:, :], func=AF.Square,
                                                                    accum_out=smB[:, n:n + 1]),
                        reads=[("bank", 5 + n)], writes=["sqB", ("ssB", n)])
                add("dve", C("tensor_tensor", out=smB[:, 2:3], in0=smB[:, 0:1], in1=smB[:, 1:2], op=ALU.add),
                    reads=[("ssB", 0), ("ssB", 1)], writes=["ssBt"])
                rstd_from_ss(smB[:, 2:3], smB[:, 3:4], smB[:, 4:5], "rstdB", "ssBt")
                for n in range(2):
                    add("dve", C("scalar_tensor_tensor",
                        out=ytmp[:, n * 512:(n + 1) * 512], in0=PS[5 + n][:, :], scalar=smB[:, 4:5],
                        in1=gBp[:, n * 512:(n + 1) * 512], op0=ALU.mult, op1=ALU.mult),
                        reads=[("bank", 5 + n), "rstdB", "gBB"], writes=[("ytmp", n)])
                add("pool", C("tensor_tensor", out=xo[xb][:, :], in0=xtB[xb][:, :], in1=ytmp[:, :],
                                                             op=ALU.add),
                    reads=[("xtB", xb), ("ytmp", 0), ("ytmp", 1)], writes=[("xoB", xb)])
                add("sp", [C("dma_start", out=x_mid[tt * 128:(tt + 1) * 128, :],
                                                                 in_=xo[xb][:, :])],
                    reads=[("xoB", xb)], writes=[("x_mid", tt)], dma=True)
        sc.barrier()

        ar.reset(small_mark)
        w1b = ar.t("w1b", [128, 8, DFF], BF16)
        w2b = ar.t("w2b", [128, 32, D], BF16)
        gB1 = ar.t("gBC1", [128, D], F32)
        gB2 = ar.t("gBC2", [128, D], F32)
        c_work = ar.mark()
        stgC = [ar.t("stgC%d" % i, [128, 2048], F32) for i in range(2)]
        ci_ = 0
        for k in range(8):
            for hf in range(2):
                b = ci_ % 2
                ci_ += 1
                add("sp", [C("dma_start",
                    out=stgC[b][:, :], in_=w_ff1[l, k * 128:(k + 1) * 128, hf * 2048:(hf + 1) * 2048])],
                    writes=[("stgC", b)], dma=True)
                convert(w1b[:, k, hf * 2048:(hf + 1) * 2048], stgC[b][:, :], [("stgC", b)], [("w1b", k, hf)])
        for m2 in range(16):
            b = ci_ % 2
            ci_ += 1
            add("sp", [C("dma_start",
                out=stgC[b][:, :].rearrange("p (a n) -> p a n", a=2),
                in_=w_ff2[l, m2 * 256:(m2 + 1) * 256, :].rearrange("(a p) n -> p a n", p=128))],
                writes=[("stgC", b)], dma=True)
            convert(w2b[:, 2 * m2:2 * m2 + 2, :].rearrange("p a n -> p (a n)"), stgC[b][:, :], [("stgC", b)],
                    [("w2b", m2)])
        add("sp", [C("dma_start", out=gB1[:, :], in_=g_pre_ffn[l:l + 1, :].to_broadcast([128, D]))],
            writes=["gBC1"], dma=True)
        add("sp", [C("dma_start", out=gB2[:, :], in_=g_post_ffn[l:l + 1, :].to_broadcast([128, D]))],
            writes=["gBC2"], dma=True)
        sc.barrier()
        ar.reset(c_work)
        NJ = CH_C // 128
        xm = [ar.t("xm%d" % i, [128, D], F32) for i in range(2 * NJ)]
        sqC = ar.t("sqC", [128, D], BF16)
        hnC = ar.t("hnC", [128, D], F32)
        hbC = ar.t("hbC", [128, D], BF16)
        h2T = [ar.t("h2T%d" % i, [128, 8, CH_C], BF16) for i in range(2)]
        a1T = ar.t("a1T", [128, 32, CH_C], BF16)
        r32 = [ar.t("r32_%d" % i, [128, CH_C], F32) for i in range(2)]
        ytC = ar.t("ytC", [128, D], F32)
        xoC = [ar.t("xoC%d" % i, [128, D], F32) for i in range(2)]
        smC = ar.t("smC", [128, 16], F32)
        NCH = S // CH_C

        def loadC(cc):
            for j in range(NJ):
                tt = cc * NJ + j
                xi = (cc % 2) * NJ + j
                add("sp", [C("dma_start", out=xm[xi][:, :],
                                                               in_=x_mid[tt * 128:(tt + 1) * 128, :])],
                    writes=[("xm", xi)], dma=True)

        loadC(0)
        for cc in range(NCH):
            if cc + 1 < NCH:
                loadC(cc + 1)
            hb_ = cc % 2
            for j in range(NJ):
                xi = (cc % 2) * NJ + j
                add("act", C("activation", out=sqC[:, :], in_=xm[xi][:, :], func=AF.Square,
                                                         accum_out=smC[:, 0:1]),
                    reads=[("xm", xi)], writes=["sqC", "ssC"])
                rstd_from_ss(smC[:, 0:1], smC[:, 1:2], smC[:, 2:3], "rstdC", "ssC")
                add("pool", C("tensor_scalar", out=hnC[:, :], in0=xm[xi][:, :], scalar1=smC[:, 2:3],
                                                             scalar2=None, op0=ALU.mult),
                    reads=[("xm", xi), "rstdC"], writes=["hnC"])
                add("pool", C("tensor_tensor", out=hbC[:, :], in0=hnC[:, :], in1=gB1[:, :], op=ALU.mult),
                    reads=["hnC", "gBC1"], writes=["hbC"])
                pT = psb(7)
                for k in range(8):
                    add("pe", C("transpose", pT[:, k * 128:(k + 1) * 128],
                                                         hbC[:, k * 128:(k + 1) * 128], ident[:, :]),
                        reads=["hbC", "ident"], writes=[("bank", 7)])
                add("act", C("activation", out=h2T[hb_][:, :, j * 128:(j + 1) * 128],
                                                       in_=pT[:, :].rearrange("p (k t) -> p k t", k=8), func=AF.Copy),
                    reads=[("bank", 7)], writes=[("h2T", hb_)])
            for m in range(32):
                bk = m % 2
                for k in range(8):
                    add("pe", C("matmul",
                        PS[bk][:, 0:CH_C], w1b[:, k, m * 128:(m + 1) * 128], h2T[hb_][:, k, :],
                        start=(k == 0), stop=(k == 7)),
                        reads=[("h2T", hb_)], writes=[("bank", bk)])
                add("act", C("activation", out=r32[bk][:, :], in_=PS[bk][:, 0:CH_C], func=AF.Relu),
                    reads=[("bank", bk)], writes=[("r32", bk)])
                en = "dve" if m % 2 == 0 else "pool"
                add(en, C("tensor_tensor", out=a1T[:, m, :], in0=r32[bk][:, :], in1=r32[bk][:, :],
                                                             op=ALU.mult),
                    reads=[("r32", bk)], writes=[("a1T", m)])
            for j in range(NJ):
                tt = cc * NJ + j
                xi = (cc % 2) * NJ + j
                ob = tt % 2
                for n in range(2):
                    pg = PS[2 + 2 * (j % 2) + n]
                    for m in range(32):
                        add("pe", C("matmul",
                            pg[:, :], a1T[:, m, j * 128:(j + 1) * 128], w2b[:, m, n * 512:(n + 1) * 512],
                            start=(m == 0), stop=(m == 31)),
                            reads=[("a1T", m)], writes=[("bank", 2 + 2 * (j % 2) + n)])
                    add("act", C("activation", out=sqC[:, 0:512], in_=pg[:, :], func=AF.Square,
                                                                  accum_out=smC[:, 4 + n:5 + n]),
                        reads=[("bank", 2 + 2 * (j % 2) + n)], writes=["sqC", ("ssC2", n)])
                add("dve", C("tensor_tensor", out=smC[:, 6:7], in0=smC[:, 4:5], in1=smC[:, 5:6], op=ALU.add),
                    reads=[("ssC2", 0), ("ssC2", 1)], writes=["ssC2t"])
                rstd_from_ss(smC[:, 6:7], smC[:, 7:8], smC[:, 8:9], "rstdC2", "ssC2t")
                for n in range(2):
                    pg = PS[2 + 2 * (j % 2) + n]
                    add("dve", C("scalar_tensor_tensor",
                        out=ytC[:, n * 512:(n + 1) * 512], in0=pg[:, :], scalar=smC[:, 8:9],
                        in1=gB2[:, n * 512:(n + 1) * 512], op0=ALU.mult, op1=ALU.mult),
                        reads=[("bank", 2 + 2 * (j % 2) + n), "rstdC2", "gBC2"], writes=[("ytC", n)])
                add("pool", C("tensor_tensor", out=xoC[ob][:, :], in0=xm[xi][:, :],
                                                                    in1=ytC[:, :], op=ALU.add),
                    reads=[("xm", xi), ("ytC", 0), ("ytC", 1)], writes=[("xoC", ob)])
                add("sp", [C("dma_start", out=x_fin[tt * 128:(tt + 1) * 128, :],
                                                                 in_=xoC[ob][:, :])],
                    reads=[("xoC", ob)], writes=[("x_fin", tt)], dma=True)
        sc.barrier()

    sc.emit(nc)
    return nc


def _consts():
    bf = ml_dtypes.bfloat16
    ident = np.eye(128, dtype=np.float32).astype(bf)
    bands = np.zeros((128, 12, 128), np.float32)
    tp = np.arange(128)[:, None]
    t = np.arange(128)[None, :]
    for g, w in enumerate((2, 4, 8, 16)):
        cur = ((tp <= t) & (tp > t - w)).astype(np.float32) / w - (tp == t)
        prev = ((tp - 128 <= t) & (tp - 128 > t - w)).astype(np.float32) / w
        cnt = np.minimum(t + 1, w).astype(np.float32)
        cur0 = ((tp <= t) & (tp > t - w)).astype(np.float32) / cnt - (tp == t)
        bands[:, 3 * g + 0, :] = cur
        bands[:, 3 * g + 1, :] = prev
        bands[:, 3 * g + 2, :] = cur0
    half = 8
    inv = (np.float32(500000.0) ** (-np.arange(half, dtype=np.float32) / np.float32(half))).astype(np.float32)
    invf = np.broadcast_to(inv[None, :], (128, 8)).copy()
    return ident, bands.astype(bf), invf


_NC_CACHE = {}


def kernel(x, positions, g_pre_mix, w_in, w_pool, pool_scale, w_out, g_post_mix,
           g_pre_ffn, w_ff1, w_ff2, g_post_ffn):
    n = 8
    if "nc" not in _NC_CACHE:
        _NC_CACHE["nc"] = build_program(DEPTH)
    nc = _NC_CACHE["nc"]
    ident, bands, invf = _consts()
    f32 = np.float32
    shared = {
        "g_pre_mix": np.ascontiguousarray(g_pre_mix, f32),
        "w_in": np.ascontiguousarray(w_in, f32),
        "w_pool": np.ascontiguousarray(w_pool, f32),
        "pool_scale_t": np.ascontiguousarray(np.asarray(pool_scale, f32).reshape(DEPTH, 4, 128).transpose(0, 2, 1)),
        "w_out": np.ascontiguousarray(w_out, f32),
        "g_post_mix": np.ascontiguousarray(g_post_mix, f32),
        "g_pre_ffn": np.ascontiguousarray(g_pre_ffn, f32),
        "w_ff1": np.ascontiguousarray(w_ff1, f32),
        "w_ff2": np.ascontiguousarray(w_ff2, f32),
        "g_post_ffn": np.ascontiguousarray(g_post_ffn, f32),
        "c_ident": ident, "c_bands": bands, "c_invf": invf,
    }
    x = np.asarray(x, f32)
    positions = np.asarray(positions, np.int32)
    in_maps = []
    for b in range(n):
        m = dict(shared)
        m["x"] = np.ascontiguousarray(x[b])
        m["pos"] = np.ascontiguousarray(positions[b].reshape(NT, 128).T)
        in_maps.append(m)
    res = run_bass_kernel_spmd(nc, in_maps, core_ids=list(range(n)))
    out = np.stack([np.asarray(r["y"], f32) for r in res.results], axis=0)
    return out
```

```python
import numpy as np
import ml_dtypes
import concourse.bass as bass
import concourse.mybir as mybir
from concourse.ap import AP
from concourse.bass_utils import run_bass_kernel_spmd

F32 = mybir.dt.float32
BF16 = mybir.dt.bfloat16
I32 = mybir.dt.int32
ALU = mybir.AluOpType
AF = mybir.ActivationFunctionType
AX = mybir.AxisListType

D = 1024
S = 4096
DEPTH = 4
INW = 2632
DFF = 4096
NT = S // 128
EPS = 1e-6
NIT = 16
TOPK = 256
NEG = -1.0e30
CH_C = 256
TWO_PI = 6.283185307179586
PI_LO = 3.1415925

ENGS = ("pe", "act", "dve", "pool", "sp")


def C(method, *args, **kwargs):
    return (method, args, kwargs)
NSLOT = 8


class Op:
    __slots__ = ("eng", "fn", "deps", "sig", "sigval", "dma", "slot", "prev", "done", "idx", "nd")

    def __init__(self, eng, fn, dma, nd):
        self.eng = eng
        self.fn = fn
        self.dma = dma
        self.nd = nd
        self.deps = []
        self.sig = False
        self.sigval = 0
        self.slot = 0
        self.prev = 0
        self.done = 0
        self.idx = 0


class Sched:
    def __init__(self):
        self.ops = {e: [] for e in ENGS}
        self.last_w = {}
        self.readers = {}
        self.dma_live = []

    def add(self, eng, fn, reads=(), writes=(), dma=False, nd=1):
        op = Op(eng, fn, dma, nd)
        op.idx = len(self.ops[eng])
        deps = {}

        def need(d, kind):
            if d is None:
                return
            if d.dma:
                deps[("d", id(d))] = d
                return
            if d.eng == eng and not dma:
                if eng == "pe" or kind == "war":
                    return
            k = ("e", d.eng)
            if k not in deps or deps[k].idx < d.idx:
                deps[k] = d

        for r in reads:
            need(self.last_w.get(r), "raw")
        for w in writes:
            need(self.last_w.get(w), "waw")
            rd = self.readers.get(w)
            if rd:
                for d in rd.values():
                    need(d, "war")
        op.deps = list(deps.values())
        for w in writes:
            self.last_w[w] = op
            self.readers[w] = {}
        for r in reads:
            key = ("d", id(op)) if dma else ("e", eng)
            self.readers.setdefault(r, {})[key] = op
        self.ops[eng].append(op)
        if dma:
            self.dma_live.append(op)
        return op

    def barrier(self):
        lasts = []
        for e in ENGS:
            for op in reversed(self.ops[e]):
                if not op.dma and op.fn is not None:
                    lasts.append(op)
                    break
        dmas = list(self.dma_live)
        for e in ENGS:
            op = Op(e, None, False, 0)
            op.idx = len(self.ops[e])
            op.deps = list(lasts) + dmas
            self.ops[e].append(op)
        self.last_w.clear()
        self.readers.clear()
        self.dma_live = []

    def emit(self, nc):
        from contextlib import ExitStack
        for e in ENGS:
            for op in self.ops[e]:
                for d in op.deps:
                    d.sig = True
        for e in ENGS:
            cnt = 0
            ndma = 0
            tot = [0] * NSLOT
            for op in self.ops[e]:
                if op.dma:
                    op.slot = ndma % NSLOT
                    ndma += 1
                    op.prev = tot[op.slot]
                    tot[op.slot] += 16 * op.nd
                    op.done = tot[op.slot]
                elif op.sig:
                    cnt += 1
                    op.sigval = cnt
        with ExitStack() as es:
            sems = {e: es.enter_context(nc.semaphore("s_" + e)) for e in ENGS}
            dsems = {}
            for e in ENGS:
                if any(op.dma for op in self.ops[e]):
                    for i in range(NSLOT):
                        dsems[(e, i)] = es.enter_context(nc.semaphore("d_%s%d" % (e, i)))
            block = es.enter_context(nc.Block())

            def run(e, eng):
                waited = {}
                for op in self.ops[e]:
                    for d in op.deps:
                        if d.dma:
                            key = ("d", d.eng, d.slot)
                            val = d.done
                            sem = dsems[(d.eng, d.slot)]
                        else:
                            key = ("e", d.eng)
                            val = d.sigval
                            sem = sems[d.eng]
                        if waited.get(key, 0) < val:
                            eng.wait_ge(sem, val)
                            waited[key] = val
                    if op.dma:
                        key = ("d", e, op.slot)
                        sem = dsems[(e, op.slot)]
                        if waited.get(key, 0) < op.prev:
                            eng.wait_ge(sem, op.prev)
                            waited[key] = op.prev
                        ins = [getattr(eng, m_)(*a_, **k_) for (m_, a_, k_) in op.fn]
                        assert len(ins) == op.nd
                        for i_ in ins:
                            i_.then_inc(sem, 16)
                    elif op.fn is not None:
                        m_, a_, k_ = op.fn
                        i_ = getattr(eng, m_)(*a_, **k_)
                        if op.sig:
                            i_.then_inc(sems[e], 1)

            @block.tensor
            def _(t):
                run("pe", t)

            @block.scalar
            def _(t):
                run("act", t)

            @block.vector
            def _(t):
                run("dve", t)

            @block.gpsimd
            def _(t):
                run("pool", t)

            @block.sync
            def _(t):
                run("sp", t)


class Arena:
    def __init__(self, nc, base, limit):
        self.nc = nc
        self.base = base
        self.off = base
        self.limit = limit
        self.n = 0

    def t(self, name, shape, dtype):
        esz = 4 if dtype in (F32, I32) else 2
        nbytes = esz
        for s_ in shape[1:]:
            nbytes *= s_
        nbytes = (nbytes + 31) // 32 * 32
        off = self.off
        self.off += nbytes
        assert self.off <= self.limit, (name, self.off, self.limit)
        self.n += 1
        return self.nc.alloc_sbuf_tensor_at("%s_%d_%d" % (name, off, self.n), list(shape), dtype, offset=off)

    def mark(self):
        return self.off

    def reset(self, off):
        self.off = off


def row_elems(t):
    n = 1
    for s_ in t.shape[1:]:
        n *= s_
    return n


def build_program(n_layers=DEPTH, debug=False, stop_after=None, n_tiles=NT, sub=99, max_ci=99, qsteps=99):
    nc = bass.Bass("TRN2", target_bir_lowering=False)
    sc = Sched()
    add = sc.add

    def din(name, shape, dt=F32):
        return nc.dram_tensor(name, list(shape), dt, kind="ExternalInput").ap()

    x_in = din("x", [S, D])
    pos_in = din("pos", [128, NT], I32)
    g_pre_mix = din("g_pre_mix", [n_layers, D])
    w_in = din("w_in", [n_layers, D, INW])
    w_pool = din("w_pool", [n_layers, 4, 128, 128])
    pscale_in = din("pool_scale_t", [n_layers, 128, 4])
    w_out = din("w_out", [n_layers, D, D])
    g_post_mix = din("g_post_mix", [n_layers, D])
    g_pre_ffn = din("g_pre_ffn", [n_layers, D])
    w_ff1 = din("w_ff1", [n_layers, D, DFF])
    w_ff2 = din("w_ff2", [n_layers, DFF, D])
    g_post_ffn = din("g_post_ffn", [n_layers, D])
    ident_in = din("c_ident", [128, 128], BF16)
    bands_in = din("c_bands", [128, 12, 128], BF16)
    invf_in = din("c_invf", [128, 8])
    y_out = nc.dram_tensor("y", [S, D], F32, kind="ExternalOutput").ap()

    skind = "ExternalOutput" if debug else "Internal"
    x_mid = nc.dram_tensor("x_mid", [S, D], F32, kind=skind).ap()
    x_nxt = nc.dram_tensor("x_nxt", [S, D], F32).ap()
    qT_s = nc.dram_tensor("qT_s", [4, 128, S], BF16, kind=skind).ap()
    aT_s = nc.dram_tensor("aT_s", [4, 128, S], BF16, kind=skind).ap()
    mT_s = nc.dram_tensor("mT_s", [NT, 128, S], BF16, kind=skind).ap()

    PS = [nc.alloc_psum_tensor("bank%d" % i, [128, 512], F32) for i in range(8)]

    def psb(i):
        return PS[i][:, :].bitcast(BF16)

    total = nc.sbuf_bytes_remaining
    guard = nc.alloc_sbuf_tensor("arena", [128, (total - 64) // 4], F32)
    base0 = int(nc.lookup_mloc(guard).addr)
    ar = Arena(nc, base0, base0 + (total - 64) // 4 * 4)

    ident = ar.t("ident", [128, 128], BF16)
    bands = ar.t("bands", [128, 12, 128], BF16)
    invf = ar.t("invf", [128, 8], F32)
    posi = ar.t("posi", [128, NT], I32)
    posf = ar.t("posf", [128, NT], F32)
    cos2 = ar.t("cos2", [128, NT, 16], F32)
    sinm = ar.t("sinm", [128, NT, 16], F32)
    pw2 = ar.t("pw2", [128, NIT + 2], F32)
    onesb = ar.t("onesb", [128, 64], BF16)
    negbig = ar.t("negbig", [128, 1], F32)
    small_mark = ar.mark()
    kT = ar.t("kT", [128, 4, S], BF16)
    Vext = ar.t("Vext", [128, NT, 520], BF16)
    phase_mark = ar.mark()

    add("sp", [C("dma_start", out=ident[:, :], in_=ident_in)], writes=["ident"], dma=True)
    add("sp", [C("dma_start", out=bands[:, :, :], in_=bands_in)], writes=["bands"], dma=True)
    add("sp", [C("dma_start", out=invf[:, :], in_=invf_in)], writes=["invf"], dma=True)
    add("sp", [C("dma_start", out=posi[:, :], in_=pos_in)], writes=["posi"], dma=True)
    for k in range(NIT + 2):
        add("pool", C("memset", pw2[:, k:k + 1], float(2.0 ** (-k))), writes=["pw2"])
    add("pool", C("memset", onesb[:, :], 1.0), writes=["onesb"])
    add("pool", C("memset", negbig[:, :], NEG), writes=["negbig"])
    m0 = ar.mark()
    ang = ar.t("ang", [128, NT, 8], F32)
    a1 = ar.t("a1", [128, NT, 8], F32)
    ki = ar.t("ki", [128, NT, 8], I32)
    kf = ar.t("kf", [128, NT, 8], F32)
    rr = ar.t("rr", [128, NT, 8], F32)
    mm = ar.t("mm", [128, NT, 8], F32)
    sn = ar.t("sn", [128, NT, 8], F32)
    add("dve", C("tensor_copy", out=posf[:, :], in_=posi[:, :]), reads=["posi"], writes=["posf"])
    posb = AP(posf, 0, [[NT, 128], [1, NT], [0, 8]])
    invb = AP(invf, 0, [[8, 128], [0, NT], [1, 8]])
    add("dve", C("tensor_tensor", out=ang[:, :, :], in0=posb, in1=invb, op=ALU.mult),
        reads=["posf", "invf"], writes=["ang"])
    for which in range(2):
        shift = np.pi if which == 0 else (np.pi + np.pi / 2)
        add("dve", C("tensor_scalar", out=a1[:, :, :], in0=ang[:, :, :], scalar1=float(shift),
                                                         scalar2=None, op0=ALU.add),
            reads=["ang"], writes=["a1"])
        add("dve", C("tensor_scalar", out=kf[:, :, :], in0=a1[:, :, :], scalar1=float(1.0 / TWO_PI),
                                             scalar2=None, op0=ALU.mult), reads=["a1"], writes=["kf"])
        add("dve", C("tensor_copy", out=ki[:, :, :], in_=kf[:, :, :]), reads=["kf"], writes=["ki"])
        add("dve", C("tensor_copy", out=kf[:, :, :], in_=ki[:, :, :]), reads=["ki"], writes=["kf"])
        add("dve", C("scalar_tensor_tensor", out=rr[:, :, :], in0=kf[:, :, :], scalar=float(-TWO_PI),
                                                    in1=a1[:, :, :], op0=ALU.mult, op1=ALU.add),
            reads=["kf", "a1"], writes=["rr"])
        add("dve", C("tensor_scalar", out=rr[:, :, :], in0=rr[:, :, :], scalar1=float(-np.pi),
                                             scalar2=None, op0=ALU.add), reads=["rr"], writes=["rr"])
        add("dve", C("tensor_scalar", out=mm[:, :, :], in0=rr[:, :, :], scalar1=float(-np.pi),
                                             scalar2=float(TWO_PI), op0=ALU.is_lt, op1=ALU.mult),
            reads=["rr"], writes=["mm"])
        add("dve", C("tensor_tensor", out=rr[:, :, :], in0=rr[:, :, :], in1=mm[:, :, :], op=ALU.add),
            reads=["rr", "mm"], writes=["rr"])
        add("dve", C("tensor_scalar", out=rr[:, :, :], in0=rr[:, :, :], scalar1=float(PI_LO),
                                             scalar2=float(-PI_LO), op0=ALU.min, op1=ALU.max),
            reads=["rr"], writes=["rr"])
        add("act", C("activation", out=sn[:, :, :], in_=rr[:, :, :], func=AF.Sin), reads=["rr"], writes=["sn"])
        if which == 0:
            add("dve", C("tensor_scalar", out=sinm[:, :, 0:8], in0=sn[:, :, :], scalar1=-1.0, scalar2=None,
                                                 op0=ALU.mult), reads=["sn"], writes=["sinm"])
            add("dve", C("tensor_copy", out=sinm[:, :, 8:16], in_=sn[:, :, :]), reads=["sn"], writes=["sinm"])
        else:
            add("dve", C("tensor_copy", out=cos2[:, :, 0:8], in_=sn[:, :, :]), reads=["sn"], writes=["cos2"])
            add("dve", C("tensor_copy", out=cos2[:, :, 8:16], in_=sn[:, :, :]), reads=["sn"], writes=["cos2"])
    sc.barrier()
    ar.reset(m0)
    if stop_after == "init":
        sc.emit(nc)
        return nc

    def rstd_from_ss(ss_ap, lnv_ap, rstd_ap, rname, sname):
        add("act", C("activation", out=lnv_ap, in_=ss_ap, func=AF.Ln, bias=float(EPS), scale=float(1.0 / D)),
            reads=[sname], writes=[rname + "_ln"])
        add("act", C("activation", out=rstd_ap, in_=lnv_ap, func=AF.Exp, scale=-0.5),
            reads=[rname + "_ln"], writes=[rname])

    conv_rr = [0]

    def convert(out_ap, in_ap, reads, writes):
        engs = ("pool", "dve", "act")
        en = engs[conv_rr[0] % 3]
        conv_rr[0] += 1
        if en == "act":
            add("act", C("activation", out=out_ap, in_=in_ap, func=AF.Copy), reads=reads, writes=writes)
        else:
            add(en, C("tensor_copy", out=out_ap, in_=in_ap), reads=reads, writes=writes)

    for l in range(n_layers):
        x_cur = x_in if l == 0 else x_nxt
        x_fin = y_out if l == n_layers - 1 else x_nxt

        ar.reset(phase_mark)
        winb = ar.t("winb", [128, 8, INW], BF16)
        wpb = ar.t("wpb", [128, 4, 128], BF16)
        ikT = ar.t("ikT", [128, S], BF16)
        a_work = ar.mark()
        add("pool", C("memset", Vext[:, :, :], 1.0), writes=["Vext_all"])
        stg = [ar.t("stgA%d" % i, [128, INW], F32) for i in range(2)]
        for k in range(8):
            b = k % 2
            add("sp", [C("dma_start", out=stg[b][:, :], in_=w_in[l, k * 128:(k + 1) * 128, :])],
                writes=[("stgA", b)], dma=True)
            h0 = 1280
            convert(winb[:, k, 0:h0], stg[b][:, 0:h0], [("stgA", b)], [("winb", k, 0)])
            convert(winb[:, k, h0:INW], stg[b][:, h0:INW], [("stgA", b)], [("winb", k, 1)])
        for g in range(4):
            b = g % 2
            add("sp", [C("dma_start", out=stg[b][:, 0:128], in_=w_pool[l, g, :, :])],
                writes=[("stgA", b)], dma=True)
            convert(wpb[:, g, :], stg[b][:, 0:128], [("stgA", b)], [("wpb", g)])
        sc.barrier()
        ar.reset(a_work)
        if stop_after == "A0":
            sc.emit(nc)
            return nc

        gB = ar.t("gBA", [128, D], F32)
        pscale = ar.t("pscale", [128, 4], F32)
        xt = [ar.t("xtA%d" % i, [128, D], F32) for i in range(2)]
        hb = ar.t("hbA", [128, D], BF16)
        sqj = hb
        hT = [ar.t("hTA%d" % i, [128, D], BF16) for i in range(2)]
        utok = [ar.t("utok%d" % i, [128, 512], BF16) for i in range(2)]
        qtok = ar.t("qtok", [128, 512], BF16)
        ktok = ar.t("ktok", [128, 512], BF16)
        iqtok = ar.t("iqtok", [128, 512], BF16)
        iktok = ar.t("iktok", [128, 128], BF16)
        qTt = [ar.t("qTt%d" % i, [128, 4, 128], BF16) for i in range(2)]
        iqTt = [ar.t("iqTt%d" % i, [128, 4, 128], BF16) for i in range(2)]
        dTb = ar.t("dTb", [128, 512], BF16)
        aTt = [ar.t("aTt%d" % i, [128, 4, 128], BF16) for i in range(2)]
        mTt = [ar.t("mTt%d" % i, [128, 8, 128], BF16) for i in range(1)]
        ropeA = ar.t("ropeA", [128, 8, 16], F32)
        ropeB = ar.t("ropeB", [128, 8, 16], F32)
        iwt = [ar.t("iwt%d" % i, [128, 8], F32) for i in range(2)]
        Dh = [ar.t("Dh%d" % i, [128, 8, 128], BF16) for i in range(2)]
        Rh = [ar.t("Rh%d" % i, [128, 512], BF16) for i in range(2)]
        scbs = [ar.t("scb%d" % i, [128, S], F32) for i in range(2)]
        mk = ar.t("mk", [128, S], BF16)
        sm = ar.t("smA", [128, 64], F32)
        hk = ar.t("hk", [128, NIT + 2], F32)
        stq = [ar.t("stq%d" % i, [128, 512], F32) for i in range(2)]
        stq_rr = [0]
        stw = ar.t("stw", [128, 72], F32)

        add("sp", [C("dma_start", out=gB[:, :], in_=g_pre_mix[l:l + 1, :].to_broadcast([128, D]))],
            writes=["gBA"], dma=True)
        add("sp", [C("dma_start", out=pscale[:, :], in_=pscale_in[l, :, :])], writes=["pscale"], dma=True)

        def loadx(tt):
            b = tt % 2
            add("sp", [C("dma_start", out=xt[b][:, :], in_=x_cur[tt * 128:(tt + 1) * 128, :])],
                writes=[("xtA", b)], dma=True)

        def rope(src_ps, dst, nh, tt, rname, wname, qs=99):
            si = stq_rr[0] % 2
            stq_rr[0] += 1
            wcols = nh * 64
            add("act", C("activation", out=stq[si][:, 0:wcols], in_=src_ps, func=AF.Copy),
                reads=[rname], writes=[("stq", si)])
            sv = stq[si][:, 0:wcols].rearrange("p (h d) -> p h d", h=nh)
            dv = dst.rearrange("p (h d) -> p h d", h=nh)
            cb = AP(cos2, tt * 16, [[NT * 16, 128], [0, nh], [1, 16]])
            s1 = AP(sinm, tt * 16, [[NT * 16, 128], [0, nh], [1, 8]])
            s2 = AP(sinm, tt * 16 + 8, [[NT * 16, 128], [0, nh], [1, 8]])
            add("pool", C("tensor_copy", out=dv[:, :, 16:64], in_=sv[:, :, 16:64]),
                reads=[("stq", si)], writes=[wname + "_r"])
            add("dve", C("tensor_tensor", out=ropeA[:, 0:nh, :], in0=sv[:, :, 0:16], in1=cb, op=ALU.mult),
                reads=[("stq", si), "cos2"], writes=["ropeA"])
            add("dve", C("tensor_tensor", out=ropeB[:, 0:nh, 0:8], in0=sv[:, :, 8:16], in1=s1, op=ALU.mult),
                reads=[("stq", si), "sinm"], writes=["ropeB0"])
            add("dve", C("tensor_tensor", out=ropeB[:, 0:nh, 8:16], in0=sv[:, :, 0:8], in1=s2, op=ALU.mult),
                reads=[("stq", si), "sinm"], writes=["ropeB1"])
            add("dve", C("tensor_tensor", out=dv[:, :, 0:16], in0=ropeA[:, 0:nh, :], in1=ropeB[:, 0:nh, :],
                                                 op=ALU.add),
                reads=["ropeA", "ropeB0", "ropeB1"], writes=[wname + "_h"])
            return si

        def rope_sb(src_sb, dst, nh, tt, rname, wname):
            sv = src_sb.rearrange("p (h d) -> p h d", h=nh)
            dv = dst.rearrange("p (h d) -> p h d", h=nh)
            cb = AP(cos2, tt * 16, [[NT * 16, 128], [0, nh], [1, 16]])
            s1 = AP(sinm, tt * 16, [[NT * 16, 128], [0, nh], [1, 8]])
            s2 = AP(sinm, tt * 16 + 8, [[NT * 16, 128], [0, nh], [1, 8]])
            add("pool", C("tensor_copy", out=dv[:, :, 16:64], in_=sv[:, :, 16:64]),
                reads=[rname], writes=[wname + "_r"])
            add("dve", C("tensor_tensor", out=ropeA[:, 0:nh, :], in0=sv[:, :, 0:16], in1=cb, op=ALU.mult),
                reads=[rname, "cos2"], writes=["ropeA"])
            add("dve", C("tensor_tensor", out=ropeB[:, 0:nh, 0:8], in0=sv[:, :, 8:16], in1=s1, op=ALU.mult),
                reads=[rname, "sinm"], writes=["ropeB0"])
            add("dve", C("tensor_tensor", out=ropeB[:, 0:nh, 8:16], in0=sv[:, :, 0:8], in1=s2, op=ALU.mult),
                reads=[rname, "sinm"], writes=["ropeB1"])
            add("dve", C("tensor_tensor", out=dv[:, :, 0:16], in0=ropeA[:, 0:nh, :], in1=ropeB[:, 0:nh, :],
                                                 op=ALU.add),
                reads=["ropeA", "ropeB0", "ropeB1"], writes=[wname + "_h"])

        CH = [(0, 512), (512, 512), (1024, 512), (1536, 512), (2048, 512), (2560, 72)]

        def stageF(tt):
            b = tt % 2
            xtb = xt[b]
            add("act", C("activation", out=sqj[:, :], in_=xtb[:, :], func=AF.Square, accum_out=sm[:, 0:1]),
                reads=[("xtA", b)], writes=["hbA", "ssA"])
            rstd_from_ss(sm[:, 0:1], sm[:, 1:2], sm[:, 2:3], "rstdA", "ssA")
            add("pool", C("tensor_scalar", out=xtb[:, :], in0=xtb[:, :], scalar1=sm[:, 2:3], scalar2=None,
                                                  op0=ALU.mult),
                reads=[("xtA", b), "rstdA"], writes=[("xtA", b)])
            add("pool", C("tensor_tensor", out=hb[:, :], in0=xtb[:, :], in1=gB[:, :], op=ALU.mult),
                reads=[("xtA", b), "gBA"], writes=["hbA"])
            if sub < 1:
                return
            pT = psb(3)
            for k in range(8):
                add("pe", C("transpose", pT[:, k * 128:(k + 1) * 128], hb[:, k * 128:(k + 1) * 128],
                                                     ident[:, :]),
                    reads=["hbA", "ident"], writes=[("bank", 3)])
            add("act", C("activation", out=hT[b][:, :], in_=pT[:, :], func=AF.Copy),
                reads=[("bank", 3)], writes=[("hTA", b)])
            if sub < 2:
                return
            for ci, (c0, wc) in enumerate(CH):
                if ci > max_ci:
                    break
                bk = ci % 3
                pj = PS[bk]
                for k in range(8):
                    add("pe", C("matmul",
                        pj[:, 0:wc], hT[b][:, k * 128:(k + 1) * 128], winb[:, k, c0:c0 + wc],
                        start=(k == 0), stop=(k == 7)),
                        reads=[("hTA", b)], writes=[("bank", bk)])
                rn = ("bank", bk)
                if ci == 0:
                    add("act", C("activation", out=utok[b][:, :], in_=pj[:, :], func=AF.Copy),
                        reads=[rn], writes=[("utok", b)])
                elif ci == 1:
                    rope(pj[:, 0:512], qtok[:, :], 8, tt, rn, "qtok")
                    pq = psb(4)
                    for c in range(4):
                        add("pe", C("transpose", pq[:, c * 128:(c + 1) * 128],
                                                             qtok[:, c * 128:(c + 1) * 128], ident[:, :]),
                            reads=["qtok_r", "qtok_h", "ident"], writes=[("bank", 4, 0)])
                    add("act", C("activation", out=qTt[b][:, :, :].rearrange("p c t -> p (c t)"),
                                                      in_=pq[:, 0:512], func=AF.Copy),
                        reads=[("bank", 4, 0)], writes=[("qTt", b)])
                    add("sp", [C("dma_start",
                        out=qT_s[:, :, tt * 128:(tt + 1) * 128].rearrange("c p t -> p c t"), in_=qTt[b][:, :, :])],
                        reads=[("qTt", b)], writes=[("qT_s", tt)], dma=True)
                elif ci == 2:
                    rope(pj[:, 0:512], ktok[:, :], 8, tt, rn, "ktok")
                    pq = psb(4)
                    for c in range(4):
                        add("pe", C("transpose", pq[:, 512 + c * 128:512 + (c + 1) * 128],
                                                             ktok[:, c * 128:(c + 1) * 128], ident[:, :]),
                            reads=["ktok_r", "ktok_h", "ident"], writes=[("bank", 4, 1)])
                    add("dve", C("tensor_copy",
                        out=kT[:, :, tt * 128:(tt + 1) * 128],
                        in_=pq[:, 512:1024].rearrange("p (c t) -> p c t", c=4)),
                        reads=[("bank", 4, 1)], writes=[("kT", tt)])
                elif ci == 3:
                    add("act", C("activation",
                        out=AP(Vext, tt * 520, [[NT * 520, 128], [65, 8], [1, 64]]),
                        in_=pj[:, 0:512].rearrange("p (h d) -> p h d", h=8), func=AF.Copy),
                        reads=[rn], writes=[("Vext", tt)])
                elif ci == 4:
                    rope(pj[:, 0:512], iqtok[:, :], 8, tt, rn, "iqtok")
                    pq = psb(3)
                    for c in range(4):
                        add("pe", C("transpose", pq[:, c * 128:(c + 1) * 128],
                                                             iqtok[:, c * 128:(c + 1) * 128], ident[:, :]),
                            reads=["iqtok_r", "iqtok_h", "ident"], writes=[("bank", 3)])
                    add("act", C("activation", out=iqTt[b][:, :, :].rearrange("p c t -> p (c t)"),
                                                      in_=pq[:, 0:512], func=AF.Copy),
                        reads=[("bank", 3)], writes=[("iqTt", b)])
                else:
                    add("act", C("activation", out=stw[:, 0:72], in_=pj[:, 0:72], func=AF.Copy),
                        reads=[rn], writes=["stw"])
                    rope_sb(stw[:, 0:64], iktok[:, 0:64], 1, tt, "stw", "iktok")
                    add("pool", C("tensor_copy", out=iktok[:, 64:128], in_=iktok[:, 0:64]),
                        reads=["iktok_r", "iktok_h"], writes=["iktok_d"])
                    add("dve", C("tensor_scalar", out=iwt[b][:, :], in0=stw[:, 64:72],
                                                                scalar1=float(8 ** -0.5 * 64 ** -0.5), scalar2=None,
                                                                op0=ALU.mult),
                        reads=["stw"], writes=[("iwt", b)])
                    pq = psb(3)
                    add("pe", C("transpose", pq[:, 512:640], iktok[:, :], ident[:, :]),
                        reads=["iktok_r", "iktok_h", "iktok_d", "ident"], writes=[("bank", 3)])
                    add("act", C("activation", out=ikT[:, tt * 128:(tt + 1) * 128], in_=pq[:, 512:640],
                                                      func=AF.Copy),
                        reads=[("bank", 3)], writes=[("ikT", tt)])
                    for h in range(8):
                        add("dve", C("tensor_scalar", out=Dh[b][:, h, :], in0=ident[:, :],
                                                                  scalar1=iwt[b][:, h:h + 1], scalar2=None,
                                                                  op0=ALU.mult),
                            reads=[("iwt", b), "ident"], writes=[("Dh", b, h)])
            if sub < 3:
                return
            pd = PS[5]
            for g in range(4):
                first = (tt == 0)
                add("pe", C("matmul",
                    pd[:, g * 128:(g + 1) * 128], utok[b][:, g * 128:(g + 1) * 128],
                    bands[:, 3 * g + (2 if first else 0), :], start=True, stop=first),
                    reads=[("utok", b), "bands"], writes=[("bank", 5)])
                if not first:
                    add("pe", C("matmul",
                        pd[:, g * 128:(g + 1) * 128], utok[1 - b][:, g * 128:(g + 1) * 128],
                        bands[:, 3 * g + 1, :], start=False, stop=True),
                        reads=[("utok", 1 - b), "bands"], writes=[("bank", 5)])
            add("act", C("activation", out=dTb[:, :], in_=pd[:, :], func=AF.Copy),
                reads=[("bank", 5)], writes=["dTb"])
            for g in range(4):
                add("pe", C("matmul", pd[:, g * 128:(g + 1) * 128], wpb[:, g, :],
                                                  dTb[:, g * 128:(g + 1) * 128], start=True, stop=True),
                    reads=["dTb"], writes=[("bank", 5)])
            for g in range(4):
                add("dve", C("tensor_scalar", out=aTt[b][:, g, :], in0=pd[:, g * 128:(g + 1) * 128],
                                                          scalar1=pscale[:, g:g + 1], scalar2=None, op0=ALU.mult),
                    reads=[("bank", 5), "pscale"], writes=[("aTt", b)])
            add("sp", [C("dma_start", out=aT_s[:, :, tt * 128:(tt + 1) * 128].rearrange("g p t -> p g t"),
                                               in_=aTt[b][:, :, :])],
                reads=[("aTt", b)], writes=[("aT_s", tt)], dma=True)
        def stageI(tt):
            b = tt % 2
            sbi = tt % 2
            NA = 128 * (tt + 1)
            nkc = (NA + 511) // 512
            items = [(kc, h) for kc in range(nkc) for h in range(8)]

            def width(kc):
                return min(512, NA - 512 * kc)

            def Lmm(i):
                kc, h = items[i]
                n = width(kc)
                r0 = 64 * (h % 2)
                pr = h // 2
                bk = 6 + (i % 2)
                kts = list(range(kc * 4, kc * 4 + (n + 127) // 128))
                add("pe", C("matmul", PS[bk][:, 0:n], iqTt[b][r0:r0 + 64, pr, :],
                                             ikT[r0:r0 + 64, kc * 512:kc * 512 + n], start=True, stop=True),
                    reads=[("iqTt", b)] + [("ikT", t_) for t_ in kts], writes=[("bank", bk)])

            def Rev(i):
                kc, h = items[i]
                n = width(kc)
                bk = 6 + (i % 2)
                rb = i % 2
                add("act", C("activation", out=Rh[rb][:, 0:n], in_=PS[bk][:, 0:n], func=AF.Relu),
                    reads=[("bank", bk)], writes=[("Rh", rb)])

            def Smm(i):
                kc, h = items[i]
                n = width(kc)
                rb = i % 2
                add("pe", C("matmul", PS[5][:, 0:n], Dh[b][:, h, :], Rh[rb][:, 0:n],
                                             start=(h == 0), stop=(h == 7)),
                    reads=[("Rh", rb), ("Dh", b, h)], writes=[("bank", 5)])
                if h == 7:
                    add("act", C("activation", out=scbs[sbi][:, kc * 512:kc * 512 + n], in_=PS[5][:, 0:n],
                                                      func=AF.Copy),
                        reads=[("bank", 5)], writes=[("scb", sbi)])

            Lmm(0)
            for i in range(len(items)):
                if i + 1 < len(items):
                    Lmm(i + 1)
                Rev(i)
                Smm(i)
        def stageB(tt):
            sbi = tt % 2
            NA = 128 * (tt + 1)
            scb = scbs[sbi]
            if tt >= 2:
                add("dve", C("tensor_reduce", out=sm[:, 8:9], in_=scb[:, 0:NA], axis=AX.X, op=ALU.max),
                    reads=[("scb", sbi)], writes=["rmax"])
                add("dve", C("tensor_reduce", out=sm[:, 9:10], in_=scb[:, 0:NA], axis=AX.X, op=ALU.min),
                    reads=[("scb", sbi)], writes=["rmin"])
            add("dve", C("memset", scb[0:64, NA - 64:NA], NEG), reads=["rmin", "rmax"] if tt >= 2 else [],
                writes=[("scb", sbi)])
            if tt >= 2:
                add("dve", C("tensor_tensor", out=sm[:, 10:11], in0=sm[:, 8:9], in1=sm[:, 9:10],
                                                     op=ALU.subtract), reads=["rmax", "rmin"], writes=["w0"])
                add("dve", C("tensor_scalar", out=sm[:, 10:11], in0=sm[:, 10:11], scalar1=1.0001,
                                                     scalar2=1e-6, op0=ALU.mult, op1=ALU.add),
                    reads=["w0"], writes=["w0"])
                add("dve", C("tensor_scalar", out=hk[:, :], in0=pw2[:, :], scalar1=sm[:, 10:11], scalar2=None,
                                                     op0=ALU.mult), reads=["w0", "pw2"], writes=["hk"])
                add("dve", C("tensor_tensor", out=sm[:, 11:12], in0=sm[:, 9:10], in1=hk[:, 1:2], op=ALU.add),
                    reads=["rmin", "hk"], writes=["mid"])
                for k in range(1, NIT + 1):
                    kn = k + 1 if k < NIT else k
                    add("dve", C("tensor_scalar", out=mk[:, 0:NA], in0=scb[:, 0:NA],
                                                              scalar1=sm[:, 11:12], scalar2=None, op0=ALU.is_ge,
                                                              op1=ALU.add, accum_out=sm[:, 12:13]),
                        reads=[("scb", sbi), "mid"], writes=["mk", "cnt"])
                    add("dve", C("tensor_tensor", out=sm[:, 13:14], in0=sm[:, 11:12],
                                                                in1=hk[:, kn:kn + 1], op=ALU.subtract),
                        reads=["mid", "hk"], writes=["tA"])
                    add("dve", C("tensor_scalar", out=sm[:, 14:15], in0=sm[:, 12:13],
                                                              scalar1=float(TOPK) - 0.5, scalar2=hk[:, k:k + 1],
                                                              op0=ALU.is_ge, op1=ALU.mult),
                        reads=["cnt", "hk"], writes=["mhk"])
                    add("dve", C("tensor_tensor", out=sm[:, 11:12], in0=sm[:, 13:14], in1=sm[:, 14:15],
                                                         op=ALU.add), reads=["tA", "mhk"], writes=["mid"])
                add("dve", C("tensor_scalar", out=mk[:, 0:NA], in0=scb[:, 0:NA], scalar1=sm[:, 11:12],
                                                     scalar2=None, op0=ALU.is_ge),
                    reads=[("scb", sbi), "mid"], writes=["mk"])
            else:
                add("dve", C("tensor_scalar", out=mk[:, 0:NA], in0=scb[:, 0:NA], scalar1=-1.0e29,
                                                     scalar2=None, op0=ALU.is_ge),
                    reads=[("scb", sbi)], writes=["mk"])

        def stageY(tt):
            nkt = tt + 1
            for g0 in range(0, nkt, 8):
                nb = min(8, nkt - g0)
                gi = 0
                pm = psb(4)
                for j in range(nb):
                    add("pe", C("transpose", pm[:, j * 128:(j + 1) * 128],
                                                         mk[:, (g0 + j) * 128:(g0 + j + 1) * 128], ident[:, :]),
                        reads=["mk", "ident"], writes=[("bank", 4, 0), ("bank", 4, 1)])
                add("act", C("activation", out=mTt[gi][:, 0:nb, :].rearrange("p k q -> p (k q)"),
                                                  in_=pm[:, 0:nb * 128], func=AF.Copy),
                    reads=[("bank", 4, 0), ("bank", 4, 1)], writes=[("mTt", gi)])
                add("sp", [C("dma_start",
                    out=mT_s[g0:g0 + nb, :, tt * 128:(tt + 1) * 128].rearrange("k p q -> p k q"),
                    in_=mTt[gi][:, 0:nb, :])],
                    reads=[("mTt", gi)], writes=[("mT_s", tt, g0)], dma=True)

        loadx(0)
        if n_tiles > 1:
            loadx(1)
        stageF(0)
        stageI(0)
        for tt in range(n_tiles):
            if tt + 1 < n_tiles:
                if tt + 2 < n_tiles:
                    loadx(tt + 2)
                stageF(tt + 1)
            stageB(tt)
            if tt + 1 < n_tiles:
                stageI(tt + 1)
            stageY(tt)
        sc.barrier()
        if stop_after == "A":
            sc.emit(nc)
            return nc

        ar.reset(phase_mark)
        woA = ar.t("woA", [128, 4, D], BF16)
        woB = ar.t("woB", [128, 8, D], BF16)
        gBp = ar.t("gBB", [128, D], F32)
        stgB = [ar.t("stgB%d" % i, [128, D], F32) for i in range(2)]
        mTc = ar.t("mTc", [128, NT, 512], BF16)
        qTc = ar.t("qTc", [128, 4, 512], BF16)
        aTc = ar.t("aTc", [128, 4, 512], BF16)
        Eb = [ar.t("Eb%d" % i, [128, 512], BF16) for i in range(3)]
        Pm = [ar.t("Pm%d" % i, [128, 512], BF16) for i in range(3)]
        boT = ar.t("boT", [128, 8, 512], BF16)
        rden = ar.t("rden", [128, 512], F32)
        rhi = ar.t("rhi", [128, 512], BF16)
        rlo = ar.t("rlo", [128, 512], BF16)
        bcs = ar.t("bcs", [128, 512], F32)
        xtB = [ar.t("xtB%d" % i, [128, D], F32) for i in range(2)]
        ytmp = ar.t("ytmp", [128, D], F32)
        xo = [ar.t("xoB%d" % i, [128, D], F32) for i in range(2)]
        sqB = ar.t("sqB", [128, 512], BF16)
        smB = ar.t("smB", [128, 16], F32)

        for g in range(4):
            b = g % 2
            add("sp", [C("dma_start", out=stgB[b][:, :], in_=w_out[l, g * 128:(g + 1) * 128, :])],
                writes=[("stgB", b)], dma=True)
            convert(woA[:, g, :], stgB[b][:, :], [("stgB", b)], [("woA", g)])
        for h in range(8):
            b = h % 2
            add("sp", [C("dma_start", out=stgB[b][0:64, :],
                                                       in_=w_out[l, 512 + 64 * h:512 + 64 * h + 64, :])],
                writes=[("stgB", b)], dma=True)
            convert(woB[0:64, h, :], stgB[b][0:64, :], [("stgB", b)], [("woB", h)])
        add("sp", [C("dma_start", out=gBp[:, :], in_=g_post_mix[l:l + 1, :].to_broadcast([128, D]))],
            writes=["gBB"], dma=True)

        for qc in range(8):
            KT = 4 * (qc + 1)
            add("sp", [C("dma_start", out=qTc[:, :, :],
                                                    in_=qT_s[:, :, qc * 512:(qc + 1) * 512].rearrange("c p t -> p c t"))],
                writes=["qTc"], dma=True)
            add("sp", [C("dma_start", out=aTc[:, :, :],
                                                    in_=aT_s[:, :, qc * 512:(qc + 1) * 512].rearrange("g p t -> p g t"))],
                writes=["aTc"], dma=True)
            for k0 in range(0, 4 * qc, 4):
                add("sp", [C("dma_start",
                    out=mTc[:, k0:k0 + 4, :],
                    in_=mT_s[k0:k0 + 4, :, qc * 512:(qc + 1) * 512].rearrange("k p q -> p k q"))],
                    writes=[("mTc", k0)], dma=True)
            for jb in range(4):
                ktb = 4 * qc + jb
                add("sp", [C("dma_start",
                    out=mTc[:, ktb, 128 * jb:512],
                    in_=mT_s[ktb, :, qc * 512 + 128 * jb:(qc + 1) * 512])],
                    writes=[("mTc", "b", ktb)], dma=True)
            cnt_i = [0]
            for h in range(8):
                r0 = 64 * (h % 2)
                pr = h // 2
                pob = 2 + (h % 2)
                po = PS[pob]

                def cstart(kt):
                    return 128 * max(0, kt - 4 * qc)

                def Smm(kt, i):
                    c0 = cstart(kt)
                    bk = i % 2
                    add("pe", C("matmul", PS[bk][:, c0:512], kT[r0:r0 + 64, pr, kt * 128:(kt + 1) * 128],
                                                 qTc[r0:r0 + 64, pr, c0:512], start=True, stop=True),
                        reads=[("kT", kt), "qTc"], writes=[("bank", bk)])

                def Ev(kt, i):
                    c0 = cstart(kt)
                    bk = i % 2
                    eb = i % 3
                    add("act", C("activation", out=Eb[eb][:, c0:512], in_=PS[bk][:, c0:512], func=AF.Exp,
                                                      scale=0.125),
                        reads=[("bank", bk)], writes=[("Eb", eb)])
                    en = "dve" if (i % 3) != 2 else "pool"
                    add(en, C("tensor_tensor", out=Pm[eb][:, c0:512], in0=Eb[eb][:, c0:512],
                                                      in1=mTc[:, kt, c0:512], op=ALU.mult),
                        reads=[("Eb", eb), (("mTc", (kt // 4) * 4) if kt < 4 * qc else ("mTc", "b", kt))], writes=[("Pm", eb)])

                def PV(kt, i):
                    c0 = cstart(kt)
                    eb = i % 3
                    add("pe", C("matmul", po[0:65, c0:512], Vext[:, kt, h * 65:h * 65 + 65],
                                                 Pm[eb][:, c0:512], start=(kt == 0), stop=(kt == KT - 1)),
                        reads=[("Pm", eb), ("Vext", kt)], writes=[("bank", pob)])

                base_i = cnt_i[0]
                Smm(0, base_i)
                for kt in range(KT):
                    if kt + 1 < KT:
                        Smm(kt + 1, base_i + kt + 1)
                    Ev(kt, base_i + kt)
                    PV(kt, base_i + kt)
                cnt_i[0] += KT
                add("dve", C("reciprocal", out=rden[64:65, :], in_=po[64:65, :]),
                    reads=[("bank", pob)], writes=["rden"])
                add("dve", C("tensor_copy", out=rhi[64:65, :], in_=rden[64:65, :]), reads=["rden"], writes=["rhi"])
                add("dve", C("tensor_tensor", out=rlo[64:65, :], in0=rden[64:65, :], in1=rhi[64:65, :],
                             op=ALU.subtract), reads=["rden", "rhi"], writes=["rlo"])
                add("pe", C("matmul", PS[4][0:64, :], onesb[64:65, 0:64], rhi[64:65, :], start=True, stop=False),
                    reads=["rhi", "onesb"], writes=[("bank", 4)])
                add("pe", C("matmul", PS[4][0:64, :], onesb[64:65, 0:64], rlo[64:65, :], start=False, stop=True),
                    reads=["rlo", "onesb"], writes=[("bank", 4)])
                add("act", C("activation", out=bcs[0:64, :], in_=PS[4][0:64, :], func=AF.Copy),
                    reads=[("bank", 4)], writes=["bcs"])
                add("dve", C("tensor_tensor", out=boT[0:64, h, :], in0=po[0:64, :],
                                                                 in1=bcs[0:64, :], op=ALU.mult),
                    reads=[("bank", pob), "bcs"], writes=[("boT", h)])
            for j in range(4):
                tt = qc * 4 + j
                xb = tt % 2
                add("sp", [C("dma_start", out=xtB[xb][:, :],
                                                               in_=x_cur[tt * 128:(tt + 1) * 128, :])],
                    writes=[("xtB", xb)], dma=True)
                for n in range(2):
                    pm_ = PS[5 + n]
                    for g in range(4):
                        add("pe", C("matmul",
                            pm_[:, :], aTc[:, g, j * 128:(j + 1) * 128], woA[:, g, n * 512:(n + 1) * 512],
                            start=(g == 0), stop=False),
                            reads=["aTc", ("woA", g)], writes=[("bank", 5 + n)])
                    for h in range(8):
                        add("pe", C("matmul",
                            pm_[:, :], boT[0:64, h, j * 128:(j + 1) * 128], woB[0:64, h, n * 512:(n + 1) * 512],
                            start=False, stop=(h == 7)),
                            reads=[("boT", h), ("woB", h)], writes=[("bank", 5 + n)])
                    add("act", C("activation", out=sqB[:, :], in_=pm_[:, :], func=AF.Square,
                                                                    accum_out=smB[:, n:n + 1]),
                        reads=[("bank", 5 + n)], writes=["sqB", ("ssB", n)])
                add("dve", C("tensor_tensor", out=smB[:, 2:3], in0=smB[:, 0:1], in1=smB[:, 1:2], op=ALU.add),
                    reads=[("ssB", 0), ("ssB", 1)], writes=["ssBt"])
                rstd_from_ss(smB[:, 2:3], smB[:, 3:4], smB[:, 4:5], "rstdB", "ssBt")
                for n in range(2):
                    add("dve", C("scalar_tensor_tensor",
                        out=ytmp[:, n * 512:(n + 1) * 512], in0=PS[5 + n][:, :], scalar=smB[:, 4:5],
                        in1=gBp[:, n * 512:(n + 1) * 512], op0=ALU.mult, op1=ALU.mult),
                        reads=[("bank", 5 + n), "rstdB", "gBB"], writes=[("ytmp", n)])
                add("pool", C("tensor_tensor", out=xo[xb][:, :], in0=xtB[xb][:, :], in1=ytmp[:, :],
                                                             op=ALU.add),
                    reads=[("xtB", xb), ("ytmp", 0), ("ytmp", 1)], writes=[("xoB", xb)])
                add("sp", [C("dma_start", out=x_mid[tt * 128:(tt + 1) * 128, :],
                                                                 in_=xo[xb][:, :])],
                    reads=[("xoB", xb)], writes=[("x_mid", tt)], dma=True)
        sc.barrier()
        if stop_after == "B":
            sc.emit(nc)
            return nc

        ar.reset(small_mark)
        w1b = ar.t("w1b", [128, 8, DFF], BF16)
        w2b = ar.t("w2b", [128, 32, D], BF16)
        gB1 = ar.t("gBC1", [128, D], F32)
        gB2 = ar.t("gBC2", [128, D], F32)
        c_work = ar.mark()
        stgC = [ar.t("stgC%d" % i, [128, 2048], F32) for i in range(2)]
        ci_ = 0
        for k in range(8):
            for hf in range(2):
                b = ci_ % 2
                ci_ += 1
                add("sp", [C("dma_start",
                    out=stgC[b][:, :], in_=w_ff1[l, k * 128:(k + 1) * 128, hf * 2048:(hf + 1) * 2048])],
                    writes=[("stgC", b)], dma=True)
                convert(w1b[:, k, hf * 2048:(hf + 1) * 2048], stgC[b][:, :], [("stgC", b)], [("w1b", k, hf)])
        for m2 in range(16):
            b = ci_ % 2
            ci_ += 1
            add("sp", [C("dma_start",
                out=stgC[b][:, :].rearrange("p (a n) -> p a n", a=2),
                in_=w_ff2[l, m2 * 256:(m2 + 1) * 256, :].rearrange("(a p) n -> p a n", p=128))],
                writes=[("stgC", b)], dma=True)
            convert(w2b[:, 2 * m2:2 * m2 + 2, :].rearrange("p a n -> p (a n)"), stgC[b][:, :], [("stgC", b)],
                    [("w2b", m2)])
        add("sp", [C("dma_start", out=gB1[:, :], in_=g_pre_ffn[l:l + 1, :].to_broadcast([128, D]))],
            writes=["gBC1"], dma=True)
        add("sp", [C("dma_start", out=gB2[:, :], in_=g_post_ffn[l:l + 1, :].to_broadcast([128, D]))],
            writes=["gBC2"], dma=True)
        sc.barrier()
        ar.reset(c_work)
        NJ = CH_C // 128
        xm = [ar.t("xm%d" % i, [128, D], F32) for i in range(2 * NJ)]
        sqC = ar.t("sqC", [128, D], BF16)
        hnC = ar.t("hnC", [128, D], F32)
        hbC = ar.t("hbC", [128, D], BF16)
        h2T = [ar.t("h2T%d" % i, [128, 8, CH_C], BF16) for i in range(2)]
        a1T = ar.t("a1T", [128, 32, CH_C], BF16)
        r32 = [ar.t("r32_%d" % i, [128, CH_C], F32) for i in range(2)]
        ytC = ar.t("ytC", [128, D], F32)
        xoC = [ar.t("xoC%d" % i, [128, D], F32) for i in range(2)]
        smC = ar.t("smC", [128, 16], F32)
        NCH = S // CH_C

        def loadC(cc):
            for j in range(NJ):
                tt = cc * NJ + j
                xi = (cc % 2) * NJ + j
                add("sp", [C("dma_start", out=xm[xi][:, :],
                                                               in_=x_mid[tt * 128:(tt + 1) * 128, :])],
                    writes=[("xm", xi)], dma=True)

        loadC(0)
        for cc in range(NCH):
            if cc + 1 < NCH:
                loadC(cc + 1)
            hb_ = cc % 2
            for j in range(NJ):
                xi = (cc % 2) * NJ + j
                add("act", C("activation", out=sqC[:, :], in_=xm[xi][:, :], func=AF.Square,
                                                         accum_out=smC[:, 0:1]),
                    reads=[("xm", xi)], writes=["sqC", "ssC"])
                rstd_from_ss(smC[:, 0:1], smC[:, 1:2], smC[:, 2:3], "rstdC", "ssC")
                add("pool", C("tensor_scalar", out=hnC[:, :], in0=xm[xi][:, :], scalar1=smC[:, 2:3],
                                                             scalar2=None, op0=ALU.mult),
                    reads=[("xm", xi), "rstdC"], writes=["hnC"])
                add("pool", C("tensor_tensor", out=hbC[:, :], in0=hnC[:, :], in1=gB1[:, :], op=ALU.mult),
                    reads=["hnC", "gBC1"], writes=["hbC"])
                pT = psb(7)
                for k in range(8):
                    add("pe", C("transpose", pT[:, k * 128:(k + 1) * 128],
                                                         hbC[:, k * 128:(k + 1) * 128], ident[:, :]),
                        reads=["hbC", "ident"], writes=[("bank", 7)])
                add("act", C("activation", out=h2T[hb_][:, :, j * 128:(j + 1) * 128],
                                                       in_=pT[:, :].rearrange("p (k t) -> p k t", k=8), func=AF.Copy),
                    reads=[("bank", 7)], writes=[("h2T", hb_)])
            for m in range(32):
                bk = m % 2
                for k in range(8):
                    add("pe", C("matmul",
                        PS[bk][:, 0:CH_C], w1b[:, k, m * 128:(m + 1) * 128], h2T[hb_][:, k, :],
                        start=(k == 0), stop=(k == 7)),
                        reads=[("h2T", hb_)], writes=[("bank", bk)])
                add("act", C("activation", out=r32[bk][:, :], in_=PS[bk][:, 0:CH_C], func=AF.Relu),
                    reads=[("bank", bk)], writes=[("r32", bk)])
                en = "dve" if m % 2 == 0 else "pool"
                add(en, C("tensor_tensor", out=a1T[:, m, :], in0=r32[bk][:, :], in1=r32[bk][:, :],
                                                             op=ALU.mult),
                    reads=[("r32", bk)], writes=[("a1T", m)])
            for j in range(NJ):
                tt = cc * NJ + j
                xi = (cc % 2) * NJ + j
                ob = tt % 2
                for n in range(2):
                    pg = PS[2 + 2 * (j % 2) + n]
                    for m in range(32):
                        add("pe", C("matmul",
                            pg[:, :], a1T[:, m, j * 128:(j + 1) * 128], w2b[:, m, n * 512:(n + 1) * 512],
                            start=(m == 0), stop=(m == 31)),
                            reads=[("a1T", m)], writes=[("bank", 2 + 2 * (j % 2) + n)])
                    add("act", C("activation", out=sqC[:, 0:512], in_=pg[:, :], func=AF.Square,
                                                                  accum_out=smC[:, 4 + n:5 + n]),
                        reads=[("bank", 2 + 2 * (j % 2) + n)], writes=["sqC", ("ssC2", n)])
                add("dve", C("tensor_tensor", out=smC[:, 6:7], in0=smC[:, 4:5], in1=smC[:, 5:6], op=ALU.add),
                    reads=[("ssC2", 0), ("ssC2", 1)], writes=["ssC2t"])
                rstd_from_ss(smC[:, 6:7], smC[:, 7:8], smC[:, 8:9], "rstdC2", "ssC2t")
                for n in range(2):
                    pg = PS[2 + 2 * (j % 2) + n]
                    add("dve", C("scalar_tensor_tensor",
                        out=ytC[:, n * 512:(n + 1) * 512], in0=pg[:, :], scalar=smC[:, 8:9],
                        in1=gB2[:, n * 512:(n + 1) * 512], op0=ALU.mult, op1=ALU.mult),
                        reads=[("bank", 2 + 2 * (j % 2) + n), "rstdC2", "gBC2"], writes=[("ytC", n)])
                add("pool", C("tensor_tensor", out=xoC[ob][:, :], in0=xm[xi][:, :],
                                                                    in1=ytC[:, :], op=ALU.add),
                    reads=[("xm", xi), ("ytC", 0), ("ytC", 1)], writes=[("xoC", ob)])
                add("sp", [C("dma_start", out=x_fin[tt * 128:(tt + 1) * 128, :],
                                                                 in_=xoC[ob][:, :])],
                    reads=[("xoC", ob)], writes=[("x_fin", tt)], dma=True)
        sc.barrier()

    sc.emit(nc)
    return nc


def _consts():
    bf = ml_dtypes.bfloat16
    ident = np.eye(128, dtype=np.float32).astype(bf)
    bands = np.zeros((128, 12, 128), np.float32)
    tp = np.arange(128)[:, None]
    t = np.arange(128)[None, :]
    for g, w in enumerate((2, 4, 8, 16)):
        cur = ((tp <= t) & (tp > t - w)).astype(np.float32) / w - (tp == t)
        prev = ((tp - 128 <= t) & (tp - 128 > t - w)).astype(np.float32) / w
        cnt = np.minimum(t + 1, w).astype(np.float32)
        cur0 = ((tp <= t) & (tp > t - w)).astype(np.float32) / cnt - (tp == t)
        bands[:, 3 * g + 0, :] = cur
        bands[:, 3 * g + 1, :] = prev
        bands[:, 3 * g + 2, :] = cur0
    half = 8
    inv = (np.float32(500000.0) ** (-np.arange(half, dtype=np.float32) / np.float32(half))).astype(np.float32)
    invf = np.broadcast_to(inv[None, :], (128, 8)).copy()
    return ident, bands.astype(bf), invf


_NC_CACHE = {}


def kernel(x, positions, g_pre_mix, w_in, w_pool, pool_scale, w_out, g_post_mix,
           g_pre_ffn, w_ff1, w_ff2, g_post_ffn):
    n = 8
    if "nc" not in _NC_CACHE:
        _NC_CACHE["nc"] = build_program(DEPTH)
    nc = _NC_CACHE["nc"]
    ident, bands, invf = _consts()
    f32 = np.float32
    shared = {
        "g_pre_mix": np.ascontiguousarray(g_pre_mix, f32),
        "w_in": np.ascontiguousarray(w_in, f32),
        "w_pool": np.ascontiguousarray(w_pool, f32),
        "pool_scale_t": np.ascontiguousarray(np.asarray(pool_scale, f32).reshape(DEPTH, 4, 128).transpose(0, 2, 1)),
        "w_out": np.ascontiguousarray(w_out, f32),
        "g_post_mix": np.ascontiguousarray(g_post_mix, f32),
        "g_pre_ffn": np.ascontiguousarray(g_pre_ffn, f32),
        "w_ff1": np.ascontiguousarray(w_ff1, f32),
        "w_ff2": np.ascontiguousarray(w_ff2, f32),
        "g_post_ffn": np.ascontiguousarray(g_post_ffn, f32),
        "c_ident": ident, "c_bands": bands, "c_invf": invf,
    }
    x = np.asarray(x, f32)
    positions = np.asarray(positions, np.int32)
    in_maps = []
    for b in range(n):
        m = dict(shared)
        m["x"] = np.ascontiguousarray(x[b])
        m["pos"] = np.ascontiguousarray(positions[b].reshape(NT, 128).T)
        in_maps.append(m)
    res = run_bass_kernel_spmd(nc, in_maps, core_ids=list(range(n)))
    out = np.stack([np.asarray(r["y"], f32) for r in res.results], axis=0)
    return out
```

```python
import numpy as np
import ml_dtypes
import concourse.bass as bass
import concourse.mybir as mybir
from concourse.ap import AP
from concourse.bass_utils import run_bass_kernel_spmd

F32 = mybir.dt.float32
BF16 = mybir.dt.bfloat16
I32 = mybir.dt.int32
ALU = mybir.AluOpType
AF = mybir.ActivationFunctionType
AX = mybir.AxisListType

D = 1024
S = 4096
DEPTH = 4
INW = 2632
DFF = 4096
NT = S // 128
EPS = 1e-6
NIT = 16
TOPK = 256
NEG = -1.0e30
CH_C = 256
TWO_PI = 6.283185307179586
PI_LO = 3.1415925

ENGS = ("pe", "act", "dve", "pool", "sp")


def C(method, *args, **kwargs):
    return (method, args, kwargs)
NSLOT = 8


class Op:
    __slots__ = ("eng", "fn", "deps", "sig", "sigval", "dma", "slot", "prev", "done", "idx", "nd")

    def __init__(self, eng, fn, dma, nd):
        self.eng = eng
        self.fn = fn
        self.dma = dma
        self.nd = nd
        self.deps = []
        self.sig = False
        self.sigval = 0
        self.slot = 0
        self.prev = 0
        self.done = 0
        self.idx = 0


class Sched:
    def __init__(self):
        self.ops = {e: [] for e in ENGS}
        self.last_w = {}
        self.readers = {}
        self.dma_live = []

    def add(self, eng, fn, reads=(), writes=(), dma=False, nd=1):
        op = Op(eng, fn, dma, nd)
        op.idx = len(self.ops[eng])
        deps = {}

        def need(d, kind):
            if d is None:
                return
            if d.dma:
                deps[("d", id(d))] = d
                return
            if d.eng == eng and not dma:
                if eng == "pe" or kind == "war":
                    return
            k = ("e", d.eng)
            if k not in deps or deps[k].idx < d.idx:
                deps[k] = d

        for r in reads:
            need(self.last_w.get(r), "raw")
        for w in writes:
            need(self.last_w.get(w), "waw")
            rd = self.readers.get(w)
            if rd:
                for d in rd.values():
                    need(d, "war")
        op.deps = list(deps.values())
        for w in writes:
            self.last_w[w] = op
            self.readers[w] = {}
        for r in reads:
            key = ("d", id(op)) if dma else ("e", eng)
            self.readers.setdefault(r, {})[key] = op
        self.ops[eng].append(op)
        if dma:
            self.dma_live.append(op)
        return op

    def barrier(self):
        lasts = []
        for e in ENGS:
            for op in reversed(self.ops[e]):
                if not op.dma and op.fn is not None:
                    lasts.append(op)
                    break
        dmas = list(self.dma_live)
        for e in ENGS:
            op = Op(e, None, False, 0)
            op.idx = len(self.ops[e])
            op.deps = list(lasts) + dmas
            self.ops[e].append(op)
        self.last_w.clear()
        self.readers.clear()
        self.dma_live = []

    def emit(self, nc):
        from contextlib import ExitStack
        for e in ENGS:
            for op in self.ops[e]:
                for d in op.deps:
                    d.sig = True
        for e in ENGS:
            cnt = 0
            ndma = 0
            tot = [0] * NSLOT
            for op in self.ops[e]:
                if op.dma:
                    op.slot = ndma % NSLOT
                    ndma += 1
                    op.prev = tot[op.slot]
                    tot[op.slot] += 16 * op.nd
                    op.done = tot[op.slot]
                elif op.sig:
                    cnt += 1
                    op.sigval = cnt
        with ExitStack() as es:
            sems = {e: es.enter_context(nc.semaphore("s_" + e)) for e in ENGS}
            dsems = {}
            for e in ENGS:
                if any(op.dma for op in self.ops[e]):
                    for i in range(NSLOT):
                        dsems[(e, i)] = es.enter_context(nc.semaphore("d_%s%d" % (e, i)))
            block = es.enter_context(nc.Block())

            def run(e, eng):
                waited = {}
                for op in self.ops[e]:
                    for d in op.deps:
                        if d.dma:
                            key = ("d", d.eng, d.slot)
                            val = d.done
                            sem = dsems[(d.eng, d.slot)]
                        else:
                            key = ("e", d.eng)
                            val = d.sigval
                            sem = sems[d.eng]
                        if waited.get(key, 0) < val:
                            eng.wait_ge(sem, val)
                            waited[key] = val
                    if op.dma:
                        key = ("d", e, op.slot)
                        sem = dsems[(e, op.slot)]
                        if waited.get(key, 0) < op.prev:
                            eng.wait_ge(sem, op.prev)
                            waited[key] = op.prev
                        ins = [getattr(eng, m_)(*a_, **k_) for (m_, a_, k_) in op.fn]
                        assert len(ins) == op.nd
                        for i_ in ins:
                            i_.then_inc(sem, 16)
                    elif op.fn is not None:
                        m_, a_, k_ = op.fn
                        i_ = getattr(eng, m_)(*a_, **k_)
                        if op.sig:
                            i_.then_inc(sems[e], 1)

            @block.tensor
            def _(t):
                run("pe", t)

            @block.scalar
            def _(t):
                run("act", t)

            @block.vector
            def _(t):
                run("dve", t)

            @block.gpsimd
            def _(t):
                run("pool", t)

            @block.sync
            def _(t):
                run("sp", t)


class Arena:
    def __init__(self, nc, base, limit):
        self.nc = nc
        self.base = base
        self.off = base
        self.limit = limit
        self.n = 0

    def t(self, name, shape, dtype):
        esz = 4 if dtype in (F32, I32) else 2
        nbytes = esz
        for s_ in shape[1:]:
            nbytes *= s_
        nbytes = (nbytes + 31) // 32 * 32
        off = self.off
        self.off += nbytes
        assert self.off <= self.limit, (name, self.off, self.limit)
        self.n += 1
        return self.nc.alloc_sbuf_tensor_at("%s_%d_%d" % (name, off, self.n), list(shape), dtype, offset=off)

    def mark(self):
        return self.off

    def reset(self, off):
        self.off = off


def row_elems(t):
    n = 1
    for s_ in t.shape[1:]:
        n *= s_
    return n


def build_program(n_layers=DEPTH, debug=False, stop_after=None, n_tiles=NT, sub=99, max_ci=99, qsteps=99):
    nc = bass.Bass("TRN2", target_bir_lowering=False)
    sc = Sched()
    add = sc.add

    def din(name, shape, dt=F32):
        return nc.dram_tensor(name, list(shape), dt, kind="ExternalInput").ap()

    x_in = din("x", [S, D])
    pos_in = din("pos", [128, NT], I32)
    g_pre_mix = din("g_pre_mix", [n_layers, D])
    w_in = din("w_in", [n_layers, D, INW])
    w_pool = din("w_pool", [n_layers, 4, 128, 128])
    pscale_in = din("pool_scale_t", [n_layers, 128, 4])
    w_out = din("w_out", [n_layers, D, D])
    g_post_mix = din("g_post_mix", [n_layers, D])
    g_pre_ffn = din("g_pre_ffn", [n_layers, D])
    w_ff1 = din("w_ff1", [n_layers, D, DFF])
    w_ff2 = din("w_ff2", [n_layers, DFF, D])
    g_post_ffn = din("g_post_ffn", [n_layers, D])
    ident_in = din("c_ident", [128, 128], BF16)
    bands_in = din("c_bands", [128, 12, 128], BF16)
    invf_in = din("c_invf", [128, 8])
    y_out = nc.dram_tensor("y", [S, D], F32, kind="ExternalOutput").ap()

    skind = "ExternalOutput" if debug else "Internal"
    x_mid = nc.dram_tensor("x_mid", [S, D], F32, kind=skind).ap()
    x_nxt = nc.dram_tensor("x_nxt", [S, D], F32).ap()
    qT_s = nc.dram_tensor("qT_s", [4, 128, S], BF16, kind=skind).ap()
    aT_s = nc.dram_tensor("aT_s", [4, 128, S], BF16, kind=skind).ap()
    mT_s = nc.dram_tensor("mT_s", [NT, 128, S], BF16, kind=skind).ap()

    PS = [nc.alloc_psum_tensor("bank%d" % i, [128, 512], F32) for i in range(8)]

    def psb(i):
        return PS[i][:, :].bitcast(BF16)

    total = nc.sbuf_bytes_remaining
    guard = nc.alloc_sbuf_tensor("arena", [128, (total - 64) // 4], F32)
    base0 = int(nc.lookup_mloc(guard).addr)
    ar = Arena(nc, base0, base0 + (total - 64) // 4 * 4)

    ident = ar.t("ident", [128, 128], BF16)
    bands = ar.t("bands", [128, 12, 128], BF16)
    invf = ar.t("invf", [128, 8], F32)
    posi = ar.t("posi", [128, NT], I32)
    posf = ar.t("posf", [128, NT], F32)
    cos2 = ar.t("cos2", [128, NT, 16], F32)
    sinm = ar.t("sinm", [128, NT, 16], F32)
    pw2 = ar.t("pw2", [128, NIT + 2], F32)
    onesb = ar.t("onesb", [128, 64], BF16)
    negbig = ar.t("negbig", [128, 1], F32)
    small_mark = ar.mark()
    kT = ar.t("kT", [128, 4, S], BF16)
    Vext = ar.t("Vext", [128, NT, 520], BF16)
    phase_mark = ar.mark()

    add("sp", [C("dma_start", out=ident[:, :], in_=ident_in)], writes=["ident"], dma=True)
    add("sp", [C("dma_start", out=bands[:, :, :], in_=bands_in)], writes=["bands"], dma=True)
    add("sp", [C("dma_start", out=invf[:, :], in_=invf_in)], writes=["invf"], dma=True)
    add("sp", [C("dma_start", out=posi[:, :], in_=pos_in)], writes=["posi"], dma=True)
    for k in range(NIT + 2):
        add("pool", C("memset", pw2[:, k:k + 1], float(2.0 ** (-k))), writes=["pw2"])
    add("pool", C("memset", onesb[:, :], 1.0), writes=["onesb"])
    add("pool", C("memset", negbig[:, :], NEG), writes=["negbig"])
    m0 = ar.mark()
    ang = ar.t("ang", [128, NT, 8], F32)
    a1 = ar.t("a1", [128, NT, 8], F32)
    ki = ar.t("ki", [128, NT, 8], I32)
    kf = ar.t("kf", [128, NT, 8], F32)
    rr = ar.t("rr", [128, NT, 8], F32)
    mm = ar.t("mm", [128, NT, 8], F32)
    sn = ar.t("sn", [128, NT, 8], F32)
    add("dve", C("tensor_copy", out=posf[:, :], in_=posi[:, :]), reads=["posi"], writes=["posf"])
    posb = AP(posf, 0, [[NT, 128], [1, NT], [0, 8]])
    invb = AP(invf, 0, [[8, 128], [0, NT], [1, 8]])
    add("dve", C("tensor_tensor", out=ang[:, :, :], in0=posb, in1=invb, op=ALU.mult),
        reads=["posf", "invf"], writes=["ang"])
    for which in range(2):
        shift = np.pi if which == 0 else (np.pi + np.pi / 2)
        add("dve", C("tensor_scalar", out=a1[:, :, :], in0=ang[:, :, :], scalar1=float(shift),
                                                         scalar2=None, op0=ALU.add),
            reads=["ang"], writes=["a1"])
        add("dve", C("tensor_scalar", out=kf[:, :, :], in0=a1[:, :, :], scalar1=float(1.0 / TWO_PI),
                                             scalar2=None, op0=ALU.mult), reads=["a1"], writes=["kf"])
        add("dve", C("tensor_copy", out=ki[:, :, :], in_=kf[:, :, :]), reads=["kf"], writes=["ki"])
        add("dve", C("tensor_copy", out=kf[:, :, :], in_=ki[:, :, :]), reads=["ki"], writes=["kf"])
        add("dve", C("scalar_tensor_tensor", out=rr[:, :, :], in0=kf[:, :, :], scalar=float(-TWO_PI),
                                                    in1=a1[:, :, :], op0=ALU.mult, op1=ALU.add),
            reads=["kf", "a1"], writes=["rr"])
        add("dve", C("tensor_scalar", out=rr[:, :, :], in0=rr[:, :, :], scalar1=float(-np.pi),
                                             scalar2=None, op0=ALU.add), reads=["rr"], writes=["rr"])
        add("dve", C("tensor_scalar", out=mm[:, :, :], in0=rr[:, :, :], scalar1=float(-np.pi),
                                             scalar2=float(TWO_PI), op0=ALU.is_lt, op1=ALU.mult),
            reads=["rr"], writes=["mm"])
        add("dve", C("tensor_tensor", out=rr[:, :, :], in0=rr[:, :, :], in1=mm[:, :, :], op=ALU.add),
            reads=["rr", "mm"], writes=["rr"])
        add("dve", C("tensor_scalar", out=rr[:, :, :], in0=rr[:, :, :], scalar1=float(PI_LO),
                                             scalar2=float(-PI_LO), op0=ALU.min, op1=ALU.max),
            reads=["rr"], writes=["rr"])
        add("act", C("activation", out=sn[:, :, :], in_=rr[:, :, :], func=AF.Sin), reads=["rr"], writes=["sn"])
        if which == 0:
            add("dve", C("tensor_scalar", out=sinm[:, :, 0:8], in0=sn[:, :, :], scalar1=-1.0, scalar2=None,
                                                 op0=ALU.mult), reads=["sn"], writes=["sinm"])
            add("dve", C("tensor_copy", out=sinm[:, :, 8:16], in_=sn[:, :, :]), reads=["sn"], writes=["sinm"])
        else:
            add("dve", C("tensor_copy", out=cos2[:, :, 0:8], in_=sn[:, :, :]), reads=["sn"], writes=["cos2"])
            add("dve", C("tensor_copy", out=cos2[:, :, 8:16], in_=sn[:, :, :]), reads=["sn"], writes=["cos2"])
    sc.barrier()
    ar.reset(m0)
    if stop_after == "init":
        sc.emit(nc)
        return nc

    def rstd_from_ss(ss_ap, lnv_ap, rstd_ap, rname, sname):
        add("act", C("activation", out=lnv_ap, in_=ss_ap, func=AF.Ln, bias=float(EPS), scale=float(1.0 / D)),
            reads=[sname], writes=[rname + "_ln"])
        add("act", C("activation", out=rstd_ap, in_=lnv_ap, func=AF.Exp, scale=-0.5),
            reads=[rname + "_ln"], writes=[rname])

    conv_rr = [0]

    def convert(out_ap, in_ap, reads, writes):
        engs = ("pool", "dve", "act")
        en = engs[conv_rr[0] % 3]
        conv_rr[0] += 1
        if en == "act":
            add("act", C("activation", out=out_ap, in_=in_ap, func=AF.Copy), reads=reads, writes=writes)
        else:
            add(en, C("tensor_copy", out=out_ap, in_=in_ap), reads=reads, writes=writes)

    for l in range(n_layers):
        x_cur = x_in if l == 0 else x_nxt
        x_fin = y_out if l == n_layers - 1 else x_nxt

        ar.reset(phase_mark)
        winb = ar.t("winb", [128, 8, INW], BF16)
        wpb = ar.t("wpb", [128, 4, 128], BF16)
        ikT = ar.t("ikT", [128, S], BF16)
        a_work = ar.mark()
        add("pool", C("memset", Vext[:, :, :], 1.0), writes=["Vext_all"])
        stg = [ar.t("stgA%d" % i, [128, INW], F32) for i in range(2)]
        for k in range(8):
            b = k % 2
            add("sp", [C("dma_start", out=stg[b][:, :], in_=w_in[l, k * 128:(k + 1) * 128, :])],
                writes=[("stgA", b)], dma=True)
            h0 = 1280
            convert(winb[:, k, 0:h0], stg[b][:, 0:h0], [("stgA", b)], [("winb", k, 0)])
            convert(winb[:, k, h0:INW], stg[b][:, h0:INW], [("stgA", b)], [("winb", k, 1)])
        for g in range(4):
            b = g % 2
            add("sp", [C("dma_start", out=stg[b][:, 0:128], in_=w_pool[l, g, :, :])],
                writes=[("stgA", b)], dma=True)
            convert(wpb[:, g, :], stg[b][:, 0:128], [("stgA", b)], [("wpb", g)])
        sc.barrier()
        ar.reset(a_work)
        if stop_after == "A0":
            sc.emit(nc)
            return nc

        gB = ar.t("gBA", [128, D], F32)
        pscale = ar.t("pscale", [128, 4], F32)
        xt = [ar.t("xtA%d" % i, [128, D], F32) for i in range(2)]
        hb = ar.t("hbA", [128, D], BF16)
        sqj = hb
        hT = [ar.t("hTA%d" % i, [128, D], BF16) for i in range(2)]
        utok = [ar.t("utok%d" % i, [128, 512], BF16) for i in range(2)]
        qtok = ar.t("qtok", [128, 512], BF16)
        ktok = ar.t("ktok", [128, 512], BF16)
        iqtok = ar.t("iqtok", [128, 512], BF16)
        iktok = ar.t("iktok", [128, 128], BF16)
        qTt = [ar.t("qTt%d" % i, [128, 4, 128], BF16) for i in range(2)]
        iqTt = [ar.t("iqTt%d" % i, [128, 4, 128], BF16) for i in range(2)]
        dTb = ar.t("dTb", [128, 512], BF16)
        aTt = [ar.t("aTt%d" % i, [128, 4, 128], BF16) for i in range(2)]
        mTt = [ar.t("mTt%d" % i, [128, 8, 128], BF16) for i in range(1)]
        ropeA = ar.t("ropeA", [128, 8, 16], F32)
        ropeB = ar.t("ropeB", [128, 8, 16], F32)
        iwt = [ar.t("iwt%d" % i, [128, 8], F32) for i in range(2)]
        Dh = [ar.t("Dh%d" % i, [128, 8, 128], BF16) for i in range(2)]
        Rh = [ar.t("Rh%d" % i, [128, 512], BF16) for i in range(2)]
        scbs = [ar.t("scb%d" % i, [128, S], F32) for i in range(2)]
        mk = ar.t("mk", [128, S], BF16)
        sm = ar.t("smA", [128, 64], F32)
        hk = ar.t("hk", [128, NIT + 2], F32)
        stq = [ar.t("stq%d" % i, [128, 512], F32) for i in range(2)]
        stq_rr = [0]
        stw = ar.t("stw", [128, 72], F32)

        add("sp", [C("dma_start", out=gB[:, :], in_=g_pre_mix[l:l + 1, :].to_broadcast([128, D]))],
            writes=["gBA"], dma=True)
        add("sp", [C("dma_start", out=pscale[:, :], in_=pscale_in[l, :, :])], writes=["pscale"], dma=True)

        def loadx(tt):
            b = tt % 2
            add("sp", [C("dma_start", out=xt[b][:, :], in_=x_cur[tt * 128:(tt + 1) * 128, :])],
                writes=[("xtA", b)], dma=True)

        def rope(src_ps, dst, nh, tt, rname, wname, qs=99):
            si = stq_rr[0] % 2
            stq_rr[0] += 1
            wcols = nh * 64
            add("act", C("activation", out=stq[si][:, 0:wcols], in_=src_ps, func=AF.Copy),
                reads=[rname], writes=[("stq", si)])
            sv = stq[si][:, 0:wcols].rearrange("p (h d) -> p h d", h=nh)
            dv = dst.rearrange("p (h d) -> p h d", h=nh)
            cb = AP(cos2, tt * 16, [[NT * 16, 128], [0, nh], [1, 16]])
            s1 = AP(sinm, tt * 16, [[NT * 16, 128], [0, nh], [1, 8]])
            s2 = AP(sinm, tt * 16 + 8, [[NT * 16, 128], [0, nh], [1, 8]])
            add("pool", C("tensor_copy", out=dv[:, :, 16:64], in_=sv[:, :, 16:64]),
                reads=[("stq", si)], writes=[wname + "_r"])
            add("dve", C("tensor_tensor", out=ropeA[:, 0:nh, :], in0=sv[:, :, 0:16], in1=cb, op=ALU.mult),
                reads=[("stq", si), "cos2"], writes=["ropeA"])
            add("dve", C("tensor_tensor", out=ropeB[:, 0:nh, 0:8], in0=sv[:, :, 8:16], in1=s1, op=ALU.mult),
                reads=[("stq", si), "sinm"], writes=["ropeB0"])
            add("dve", C("tensor_tensor", out=ropeB[:, 0:nh, 8:16], in0=sv[:, :, 0:8], in1=s2, op=ALU.mult),
                reads=[("stq", si), "sinm"], writes=["ropeB1"])
            add("dve", C("tensor_tensor", out=dv[:, :, 0:16], in0=ropeA[:, 0:nh, :], in1=ropeB[:, 0:nh, :],
                                                 op=ALU.add),
                reads=["ropeA", "ropeB0", "ropeB1"], writes=[wname + "_h"])
            return si

        def rope_sb(src_sb, dst, nh, tt, rname, wname):
            sv = src_sb.rearrange("p (h d) -> p h d", h=nh)
            dv = dst.rearrange("p (h d) -> p h d", h=nh)
            cb = AP(cos2, tt * 16, [[NT * 16, 128], [0, nh], [1, 16]])
            s1 = AP(sinm, tt * 16, [[NT * 16, 128], [0, nh], [1, 8]])
            s2 = AP(sinm, tt * 16 + 8, [[NT * 16, 128], [0, nh], [1, 8]])
            add("pool", C("tensor_copy", out=dv[:, :, 16:64], in_=sv[:, :, 16:64]),
                reads=[rname], writes=[wname + "_r"])
            add("dve", C("tensor_tensor", out=ropeA[:, 0:nh, :], in0=sv[:, :, 0:16], in1=cb, op=ALU.mult),
                reads=[rname, "cos2"], writes=["ropeA"])
            add("dve", C("tensor_tensor", out=ropeB[:, 0:nh, 0:8], in0=sv[:, :, 8:16], in1=s1, op=ALU.mult),
                reads=[rname, "sinm"], writes=["ropeB0"])
            add("dve", C("tensor_tensor", out=ropeB[:, 0:nh, 8:16], in0=sv[:, :, 0:8], in1=s2, op=ALU.mult),
                reads=[rname, "sinm"], writes=["ropeB1"])
            add("dve", C("tensor_tensor", out=dv[:, :, 0:16], in0=ropeA[:, 0:nh, :], in1=ropeB[:, 0:nh, :],
                                                 op=ALU.add),
                reads=["ropeA", "ropeB0", "ropeB1"], writes=[wname + "_h"])

        CH = [(0, 512), (512, 512), (1024, 512), (1536, 512), (2048, 512), (2560, 72)]

        def stageF(tt):
            b = tt % 2
            xtb = xt[b]
            add("act", C("activation", out=sqj[:, :], in_=xtb[:, :], func=AF.Square, accum_out=sm[:, 0:1]),
                reads=[("xtA", b)], writes=["hbA", "ssA"])
            rstd_from_ss(sm[:, 0:1], sm[:, 1:2], sm[:, 2:3], "rstdA", "ssA")
            add("pool", C("tensor_scalar", out=xtb[:, :], in0=xtb[:, :], scalar1=sm[:, 2:3], scalar2=None,
                                                  op0=ALU.mult),
                reads=[("xtA", b), "rstdA"], writes=[("xtA", b)])
            add("pool", C("tensor_tensor", out=hb[:, :], in0=xtb[:, :], in1=gB[:, :], op=ALU.mult),
                reads=[("xtA", b), "gBA"], writes=["hbA"])
            if sub < 1:
                return
            pT = psb(3)
            for k in range(8):
                add("pe", C("transpose", pT[:, k * 128:(k + 1) * 128], hb[:, k * 128:(k + 1) * 128],
                                                     ident[:, :]),
                    reads=["hbA", "ident"], writes=[("bank", 3)])
            add("act", C("activation", out=hT[b][:, :], in_=pT[:, :], func=AF.Copy),
                reads=[("bank", 3)], writes=[("hTA", b)])
            if sub < 2:
                return
            for ci, (c0, wc) in enumerate(CH):
                if ci > max_ci:
                    break
                bk = ci % 3
                pj = PS[bk]
                for k in range(8):
                    add("pe", C("matmul",
                        pj[:, 0:wc], hT[b][:, k * 128:(k + 1) * 128], winb[:, k, c0:c0 + wc],
                        start=(k == 0), stop=(k == 7)),
                        reads=[("hTA", b)], writes=[("bank", bk)])
                rn = ("bank", bk)
                if ci == 0:
                    add("act", C("activation", out=utok[b][:, :], in_=pj[:, :], func=AF.Copy),
                        reads=[rn], writes=[("utok", b)])
                elif ci == 1:
                    rope(pj[:, 0:512], qtok[:, :], 8, tt, rn, "qtok")
                    pq = psb(4)
                    for c in range(4):
                        add("pe", C("transpose", pq[:, c * 128:(c + 1) * 128],
                                                             qtok[:, c * 128:(c + 1) * 128], ident[:, :]),
                            reads=["qtok_r", "qtok_h", "ident"], writes=[("bank", 4, 0)])
                    add("act", C("activation", out=qTt[b][:, :, :].rearrange("p c t -> p (c t)"),
                                                      in_=pq[:, 0:512], func=AF.Copy),
                        reads=[("bank", 4, 0)], writes=[("qTt", b)])
                    add("sp", [C("dma_start",
                        out=qT_s[:, :, tt * 128:(tt + 1) * 128].rearrange("c p t -> p c t"), in_=qTt[b][:, :, :])],
                        reads=[("qTt", b)], writes=[("qT_s", tt)], dma=True)
                elif ci == 2:
                    rope(pj[:, 0:512], ktok[:, :], 8, tt, rn, "ktok")
                    pq = psb(4)
                    for c in range(4):
                        add("pe", C("transpose", pq[:, 512 + c * 128:512 + (c + 1) * 128],
                                                             ktok[:, c * 128:(c + 1) * 128], ident[:, :]),
                            reads=["ktok_r", "ktok_h", "ident"], writes=[("bank", 4, 1)])
                    add("dve", C("tensor_copy",
                        out=kT[:, :, tt * 128:(tt + 1) * 128],
                        in_=pq[:, 512:1024].rearrange("p (c t) -> p c t", c=4)),
                        reads=[("bank", 4, 1)], writes=[("kT", tt)])
                elif ci == 3:
                    add("act", C("activation",
                        out=AP(Vext, tt * 520, [[NT * 520, 128], [65, 8], [1, 64]]),
                        in_=pj[:, 0:512].rearrange("p (h d) -> p h d", h=8), func=AF.Copy),
                        reads=[rn], writes=[("Vext", tt)])
                elif ci == 4:
                    rope(pj[:, 0:512], iqtok[:, :], 8, tt, rn, "iqtok")
                    pq = psb(3)
                    for c in range(4):
                        add("pe", C("transpose", pq[:, c * 128:(c + 1) * 128],
                                                             iqtok[:, c * 128:(c + 1) * 128], ident[:, :]),
                            reads=["iqtok_r", "iqtok_h", "ident"], writes=[("bank", 3)])
                    add("act", C("activation", out=iqTt[b][:, :, :].rearrange("p c t -> p (c t)"),
                                                      in_=pq[:, 0:512], func=AF.Copy),
                        reads=[("bank", 3)], writes=[("iqTt", b)])
                else:
                    add("act", C("activation", out=stw[:, 0:72], in_=pj[:, 0:72], func=AF.Copy),
                        reads=[rn], writes=["stw"])
                    rope_sb(stw[:, 0:64], iktok[:, 0:64], 1, tt, "stw", "iktok")
                    add("pool", C("tensor_copy", out=iktok[:, 64:128], in_=iktok[:, 0:64]),
                        reads=["iktok_r", "iktok_h"], writes=["iktok_d"])
                    add("dve", C("tensor_scalar", out=iwt[b][:, :], in0=stw[:, 64:72],
                                                                scalar1=float(8 ** -0.5 * 64 ** -0.5), scalar2=None,
                                                                op0=ALU.mult),
                        reads=["stw"], writes=[("iwt", b)])
                    pq = psb(3)
                    add("pe", C("transpose", pq[:, 512:640], iktok[:, :], ident[:, :]),
                        reads=["iktok_r", "iktok_h", "iktok_d", "ident"], writes=[("bank", 3)])
                    add("act", C("activation", out=ikT[:, tt * 128:(tt + 1) * 128], in_=pq[:, 512:640],
                                                      func=AF.Copy),
                        reads=[("bank", 3)], writes=[("ikT", tt)])
                    for h in range(8):
                        add("dve", C("tensor_scalar", out=Dh[b][:, h, :], in0=ident[:, :],
                                                                  scalar1=iwt[b][:, h:h + 1], scalar2=None,
                                                                  op0=ALU.mult),
                            reads=[("iwt", b), "ident"], writes=[("Dh", b, h)])
            if sub < 3:
                return
            pd = PS[5]
            for g in range(4):
                first = (tt == 0)
                add("pe", C("matmul",
                    pd[:, g * 128:(g + 1) * 128], utok[b][:, g * 128:(g + 1) * 128],
                    bands[:, 3 * g + (2 if first else 0), :], start=True, stop=first),
                    reads=[("utok", b), "bands"], writes=[("bank", 5)])
                if not first:
                    add("pe", C("matmul",
                        pd[:, g * 128:(g + 1) * 128], utok[1 - b][:, g * 128:(g + 1) * 128],
                        bands[:, 3 * g + 1, :], start=False, stop=True),
                        reads=[("utok", 1 - b), "bands"], writes=[("bank", 5)])
            add("act", C("activation", out=dTb[:, :], in_=pd[:, :], func=AF.Copy),
                reads=[("bank", 5)], writes=["dTb"])
            for g in range(4):
                add("pe", C("matmul", pd[:, g * 128:(g + 1) * 128], wpb[:, g, :],
                                                  dTb[:, g * 128:(g + 1) * 128], start=True, stop=True),
                    reads=["dTb"], writes=[("bank", 5)])
            for g in range(4):
                add("dve", C("tensor_scalar", out=aTt[b][:, g, :], in0=pd[:, g * 128:(g + 1) * 128],
                                                          scalar1=pscale[:, g:g + 1], scalar2=None, op0=ALU.mult),
                    reads=[("bank", 5), "pscale"], writes=[("aTt", b)])
            add("sp", [C("dma_start", out=aT_s[:, :, tt * 128:(tt + 1) * 128].rearrange("g p t -> p g t"),
                                               in_=aTt[b][:, :, :])],
                reads=[("aTt", b)], writes=[("aT_s", tt)], dma=True)
        def stageI(tt):
            b = tt % 2
            sbi = tt % 2
            NA = 128 * (tt + 1)
            nkc = (NA + 511) // 512
            items = [(kc, h) for kc in range(nkc) for h in range(8)]

            def width(kc):
                return min(512, NA - 512 * kc)

            def Lmm(i):
                kc, h = items[i]
                n = width(kc)
                r0 = 64 * (h % 2)
                pr = h // 2
                bk = 6 + (i % 2)
                kts = list(range(kc * 4, kc * 4 + (n + 127) // 128))
                add("pe", C("matmul", PS[bk][:, 0:n], iqTt[b][r0:r0 + 64, pr, :],
                                             ikT[r0:r0 + 64, kc * 512:kc * 512 + n], start=True, stop=True),
                    reads=[("iqTt", b)] + [("ikT", t_) for t_ in kts], writes=[("bank", bk)])

            def Rev(i):
                kc, h = items[i]
                n = width(kc)
                bk = 6 + (i % 2)
                rb = i % 2
                add("act", C("activation", out=Rh[rb][:, 0:n], in_=PS[bk][:, 0:n], func=AF.Relu),
                    reads=[("bank", bk)], writes=[("Rh", rb)])

            def Smm(i):
                kc, h = items[i]
                n = width(kc)
                rb = i % 2
                add("pe", C("matmul", PS[5][:, 0:n], Dh[b][:, h, :], Rh[rb][:, 0:n],
                                             start=(h == 0), stop=(h == 7)),
                    reads=[("Rh", rb), ("Dh", b, h)], writes=[("bank", 5)])
                if h == 7:
                    add("act", C("activation", out=scbs[sbi][:, kc * 512:kc * 512 + n], in_=PS[5][:, 0:n],
                                                      func=AF.Copy),
                        reads=[("bank", 5)], writes=[("scb", sbi)])

            Lmm(0)
            for i in range(len(items)):
                if i + 1 < len(items):
                    Lmm(i + 1)
                Rev(i)
                Smm(i)
        def stageB(tt):
            sbi = tt % 2
            NA = 128 * (tt + 1)
            scb = scbs[sbi]
            if tt >= 2:
                add("dve", C("tensor_reduce", out=sm[:, 8:9], in_=scb[:, 0:NA], axis=AX.X, op=ALU.max),
                    reads=[("scb", sbi)], writes=["rmax"])
                add("dve", C("tensor_reduce", out=sm[:, 9:10], in_=scb[:, 0:NA], axis=AX.X, op=ALU.min),
                    reads=[("scb", sbi)], writes=["rmin"])
            add("dve", C("memset", scb[0:64, NA - 64:NA], NEG), reads=["rmin", "rmax"] if tt >= 2 else [],
                writes=[("scb", sbi)])
            if tt >= 2:
                add("dve", C("tensor_tensor", out=sm[:, 10:11], in0=sm[:, 8:9], in1=sm[:, 9:10],
                                                     op=ALU.subtract), reads=["rmax", "rmin"], writes=["w0"])
                add("dve", C("tensor_scalar", out=sm[:, 10:11], in0=sm[:, 10:11], scalar1=1.0001,
                                                     scalar2=1e-6, op0=ALU.mult, op1=ALU.add),
                    reads=["w0"], writes=["w0"])
                add("dve", C("tensor_scalar", out=hk[:, :], in0=pw2[:, :], scalar1=sm[:, 10:11], scalar2=None,
                                                     op0=ALU.mult), reads=["w0", "pw2"], writes=["hk"])
                add("dve", C("tensor_tensor", out=sm[:, 11:12], in0=sm[:, 9:10], in1=hk[:, 1:2], op=ALU.add),
                    reads=["rmin", "hk"], writes=["mid"])
                for k in range(1, NIT + 1):
                    kn = k + 1 if k < NIT else k
                    add("dve", C("tensor_scalar", out=mk[:, 0:NA], in0=scb[:, 0:NA],
                                                              scalar1=sm[:, 11:12], scalar2=None, op0=ALU.is_ge,
                                                              op1=ALU.add, accum_out=sm[:, 12:13]),
                        reads=[("scb", sbi), "mid"], writes=["mk", "cnt"])
                    add("dve", C("tensor_tensor", out=sm[:, 13:14], in0=sm[:, 11:12],
                                                                in1=hk[:, kn:kn + 1], op=ALU.subtract),
                        reads=["mid", "hk"], writes=["tA"])
                    add("dve", C("tensor_scalar", out=sm[:, 14:15], in0=sm[:, 12:13],
                                                              scalar1=float(TOPK) - 0.5, scalar2=hk[:, k:k + 1],
                                                              op0=ALU.is_ge, op1=ALU.mult),
                        reads=["cnt", "hk"], writes=["mhk"])
                    add("dve", C("tensor_tensor", out=sm[:, 11:12], in0=sm[:, 13:14], in1=sm[:, 14:15],
                                                         op=ALU.add), reads=["tA", "mhk"], writes=["mid"])
                add("dve", C("tensor_scalar", out=mk[:, 0:NA], in0=scb[:, 0:NA], scalar1=sm[:, 11:12],
                                                     scalar2=None, op0=ALU.is_ge),
                    reads=[("scb", sbi), "mid"], writes=["mk"])
            else:
                add("dve", C("tensor_scalar", out=mk[:, 0:NA], in0=scb[:, 0:NA], scalar1=-1.0e29,
                                                     scalar2=None, op0=ALU.is_ge),
                    reads=[("scb", sbi)], writes=["mk"])

        def stageY(tt):
            nkt = tt + 1
            for g0 in range(0, nkt, 8):
                nb = min(8, nkt - g0)
                gi = 0
                pm = psb(4)
                for j in range(nb):
                    add("pe", C("transpose", pm[:, j * 128:(j + 1) * 128],
                                                         mk[:, (g0 + j) * 128:(g0 + j + 1) * 128], ident[:, :]),
                        reads=["mk", "ident"], writes=[("bank", 4, 0), ("bank", 4, 1)])
                add("act", C("activation", out=mTt[gi][:, 0:nb, :].rearrange("p k q -> p (k q)"),
                                                  in_=pm[:, 0:nb * 128], func=AF.Copy),
                    reads=[("bank", 4, 0), ("bank", 4, 1)], writes=[("mTt", gi)])
                add("sp", [C("dma_start",
                    out=mT_s[g0:g0 + nb, :, tt * 128:(tt + 1) * 128].rearrange("k p q -> p k q"),
                    in_=mTt[gi][:, 0:nb, :])],
                    reads=[("mTt", gi)], writes=[("mT_s", tt, g0)], dma=True)

        loadx(0)
        if n_tiles > 1:
            loadx(1)
        stageF(0)
        stageI(0)
        if n_tiles > 1:
            if n_tiles > 2:
                loadx(2)
            stageF(1)
        for tt in range(n_tiles):
            if tt + 2 < n_tiles:
                if tt + 3 < n_tiles:
                    loadx(tt + 3)
                stageF(tt + 2)
            stageB(tt)
            if tt + 1 < n_tiles:
                stageI(tt + 1)
            stageY(tt)
        sc.barrier()
        if stop_after == "A":
            sc.emit(nc)
            return nc

        ar.reset(phase_mark)
        woA = ar.t("woA", [128, 4, D], BF16)
        woB = ar.t("woB", [128, 8, D], BF16)
        gBp = ar.t("gBB", [128, D], F32)
        stgB = [ar.t("stgB%d" % i, [128, D], F32) for i in range(2)]
        mTc = ar.t("mTc", [128, NT, 512], BF16)
        qTc = ar.t("qTc", [128, 4, 512], BF16)
        aTc = ar.t("aTc", [128, 4, 512], BF16)
        Eb = [ar.t("Eb%d" % i, [128, 512], BF16) for i in range(3)]
        Pm = [ar.t("Pm%d" % i, [128, 512], BF16) for i in range(3)]
        boT = ar.t("boT", [128, 8, 512], BF16)
        rden = ar.t("rden", [128, 512], F32)
        rhi = ar.t("rhi", [128, 512], BF16)
        rlo = ar.t("rlo", [128, 512], BF16)
        bcs = ar.t("bcs", [128, 512], F32)
        xtB = [ar.t("xtB%d" % i, [128, D], F32) for i in range(2)]
        ytmp = ar.t("ytmp", [128, D], F32)
        xo = [ar.t("xoB%d" % i, [128, D], F32) for i in range(2)]
        sqB = ar.t("sqB", [128, 512], BF16)
        smB = ar.t("smB", [128, 16], F32)

        for g in range(4):
            b = g % 2
            add("sp", [C("dma_start", out=stgB[b][:, :], in_=w_out[l, g * 128:(g + 1) * 128, :])],
                writes=[("stgB", b)], dma=True)
            convert(woA[:, g, :], stgB[b][:, :], [("stgB", b)], [("woA", g)])
        for h in range(8):
            b = h % 2
            add("sp", [C("dma_start", out=stgB[b][0:64, :],
                                                       in_=w_out[l, 512 + 64 * h:512 + 64 * h + 64, :])],
                writes=[("stgB", b)], dma=True)
            convert(woB[0:64, h, :], stgB[b][0:64, :], [("stgB", b)], [("woB", h)])
        add("sp", [C("dma_start", out=gBp[:, :], in_=g_post_mix[l:l + 1, :].to_broadcast([128, D]))],
            writes=["gBB"], dma=True)

        for qc in range(8):
            KT = 4 * (qc + 1)
            add("sp", [C("dma_start", out=qTc[:, :, :],
                                                    in_=qT_s[:, :, qc * 512:(qc + 1) * 512].rearrange("c p t -> p c t"))],
                writes=["qTc"], dma=True)
            add("sp", [C("dma_start", out=aTc[:, :, :],
                                                    in_=aT_s[:, :, qc * 512:(qc + 1) * 512].rearrange("g p t -> p g t"))],
                writes=["aTc"], dma=True)
            for k0 in range(0, 4 * qc, 4):
                add("sp", [C("dma_start",
                    out=mTc[:, k0:k0 + 4, :],
                    in_=mT_s[k0:k0 + 4, :, qc * 512:(qc + 1) * 512].rearrange("k p q -> p k q"))],
                    writes=[("mTc", k0)], dma=True)
            for jb in range(4):
                ktb = 4 * qc + jb
                add("sp", [C("dma_start",
                    out=mTc[:, ktb, 128 * jb:512],
                    in_=mT_s[ktb, :, qc * 512 + 128 * jb:(qc + 1) * 512])],
                    writes=[("mTc", "b", ktb)], dma=True)
            cnt_i = [0]
            for h in range(8):
                r0 = 64 * (h % 2)
                pr = h // 2
                pob = 2 + (h % 2)
                po = PS[pob]

                def cstart(kt):
                    return 128 * max(0, kt - 4 * qc)

                def Smm(kt, i):
                    c0 = cstart(kt)
                    bk = i % 2
                    add("pe", C("matmul", PS[bk][:, c0:512], kT[r0:r0 + 64, pr, kt * 128:(kt + 1) * 128],
                                                 qTc[r0:r0 + 64, pr, c0:512], start=True, stop=True),
                        reads=[("kT", kt), "qTc"], writes=[("bank", bk)])

                def Ev(kt, i):
                    c0 = cstart(kt)
                    bk = i % 2
                    eb = i % 3
                    add("act", C("activation", out=Eb[eb][:, c0:512], in_=PS[bk][:, c0:512], func=AF.Exp,
                                                      scale=0.125),
                        reads=[("bank", bk)], writes=[("Eb", eb)])
                    en = "dve"
                    add(en, C("tensor_tensor", out=Pm[eb][:, c0:512], in0=Eb[eb][:, c0:512],
                                                      in1=mTc[:, kt, c0:512], op=ALU.mult),
                        reads=[("Eb", eb), (("mTc", (kt // 4) * 4) if kt < 4 * qc else ("mTc", "b", kt))], writes=[("Pm", eb)])

                def PV(kt, i):
                    c0 = cstart(kt)
                    eb = i % 3
                    add("pe", C("matmul", po[0:65, c0:512], Vext[:, kt, h * 65:h * 65 + 65],
                                                 Pm[eb][:, c0:512], start=(kt == 0), stop=(kt == KT - 1)),
                        reads=[("Pm", eb), ("Vext", kt)], writes=[("bank", pob)])

                base_i = cnt_i[0]
                Smm(0, base_i)
                for kt in range(KT):
                    if kt + 1 < KT:
                        Smm(kt + 1, base_i + kt + 1)
                    Ev(kt, base_i + kt)
                    PV(kt, base_i + kt)
                cnt_i[0] += KT
                add("dve", C("reciprocal", out=rden[64:65, :], in_=po[64:65, :]),
                    reads=[("bank", pob)], writes=["rden"])
                add("dve", C("tensor_copy", out=rhi[64:65, :], in_=rden[64:65, :]), reads=["rden"], writes=["rhi"])
                add("dve", C("tensor_tensor", out=rlo[64:65, :], in0=rden[64:65, :], in1=rhi[64:65, :],
                             op=ALU.subtract), reads=["rden", "rhi"], writes=["rlo"])
                add("pe", C("matmul", PS[4][0:64, :], onesb[64:65, 0:64], rhi[64:65, :], start=True, stop=False),
                    reads=["rhi", "onesb"], writes=[("bank", 4)])
                add("pe", C("matmul", PS[4][0:64, :], onesb[64:65, 0:64], rlo[64:65, :], start=False, stop=True),
                    reads=["rlo", "onesb"], writes=[("bank", 4)])
                add("act", C("activation", out=bcs[0:64, :], in_=PS[4][0:64, :], func=AF.Copy),
                    reads=[("bank", 4)], writes=["bcs"])
                add("dve", C("tensor_tensor", out=boT[0:64, h, :], in0=po[0:64, :],
                                                                 in1=bcs[0:64, :], op=ALU.mult),
                    reads=[("bank", pob), "bcs"], writes=[("boT", h)])
            for j in range(4):
                tt = qc * 4 + j
                xb = tt % 2
                add("sp", [C("dma_start", out=xtB[xb][:, :],
                                                               in_=x_cur[tt * 128:(tt + 1) * 128, :])],
                    writes=[("xtB", xb)], dma=True)
                for n in range(2):
                    pm_ = PS[5 + n]
                    for g in range(4):
                        add("pe", C("matmul",
                            pm_[:, :], aTc[:, g, j * 128:(j + 1) * 128], woA[:, g, n * 512:(n + 1) * 512],
                            start=(g == 0), stop=False),
                            reads=["aTc", ("woA", g)], writes=[("bank", 5 + n)])
                    for h in range(8):
                        add("pe", C("matmul",
                            pm_[:, :], boT[0:64, h, j * 128:(j + 1) * 128], woB[0:64, h, n * 512:(n + 1) * 512],
                            start=False, stop=(h == 7)),
                            reads=[("boT", h), ("woB", h)], writes=[("bank", 5 + n)])
                    add("act", C("activation", out=sqB[:, :], in_=pm_[:, :], func=AF.Square,
                                                                    accum_out=smB[:, n:n + 1]),
                        reads=[("bank", 5 + n)], writes=["sqB", ("ssB", n)])
                add("dve", C("tensor_tensor", out=smB[:, 2:3], in0=smB[:, 0:1], in1=smB[:, 1:2], op=ALU.add),
                    reads=[("ssB", 0), ("ssB", 1)], writes=["ssBt"])
                rstd_from_ss(smB[:, 2:3], smB[:, 3:4], smB[:, 4:5], "rstdB", "ssBt")
                for n in range(2):
                    add("dve", C("scalar_tensor_tensor",
                        out=ytmp[:, n * 512:(n + 1) * 512], in0=PS[5 + n][:, :], scalar=smB[:, 4:5],
                        in1=gBp[:, n * 512:(n + 1) * 512], op0=ALU.mult, op1=ALU.mult),
                        reads=[("bank", 5 + n), "rstdB", "gBB"], writes=[("ytmp", n)])
                add("pool", C("tensor_tensor", out=xo[xb][:, :], in0=xtB[xb][:, :], in1=ytmp[:, :],
                                                             op=ALU.add),
                    reads=[("xtB", xb), ("ytmp", 0), ("ytmp", 1)], writes=[("xoB", xb)])
                add("sp", [C("dma_start", out=x_mid[tt * 128:(tt + 1) * 128, :],
                                                                 in_=xo[xb][:, :])],
                    reads=[("xoB", xb)], writes=[("x_mid", tt)], dma=True)
        sc.barrier()
        if stop_after == "B":
            sc.emit(nc)
            return nc

        ar.reset(small_mark)
        w1b = ar.t("w1b", [128, 8, DFF], BF16)
        w2b = ar.t("w2b", [128, 32, D], BF16)
        gB1 = ar.t("gBC1", [128, D], F32)
        gB2 = ar.t("gBC2", [128, D], F32)
        c_work = ar.mark()
        stgC = [ar.t("stgC%d" % i, [128, 2048], F32) for i in range(2)]
        ci_ = 0
        for k in range(8):
            for hf in range(2):
                b = ci_ % 2
                ci_ += 1
                add("sp", [C("dma_start",
                    out=stgC[b][:, :], in_=w_ff1[l, k * 128:(k + 1) * 128, hf * 2048:(hf + 1) * 2048])],
                    writes=[("stgC", b)], dma=True)
                convert(w1b[:, k, hf * 2048:(hf + 1) * 2048], stgC[b][:, :], [("stgC", b)], [("w1b", k, hf)])
        for m2 in range(16):
            b = ci_ % 2
            ci_ += 1
            add("sp", [C("dma_start",
                out=stgC[b][:, :].rearrange("p (a n) -> p a n", a=2),
                in_=w_ff2[l, m2 * 256:(m2 + 1) * 256, :].rearrange("(a p) n -> p a n", p=128))],
                writes=[("stgC", b)], dma=True)
            convert(w2b[:, 2 * m2:2 * m2 + 2, :].rearrange("p a n -> p (a n)"), stgC[b][:, :], [("stgC", b)],
                    [("w2b", m2)])
        add("sp", [C("dma_start", out=gB1[:, :], in_=g_pre_ffn[l:l + 1, :].to_broadcast([128, D]))],
            writes=["gBC1"], dma=True)
        add("sp", [C("dma_start", out=gB2[:, :], in_=g_post_ffn[l:l + 1, :].to_broadcast([128, D]))],
            writes=["gBC2"], dma=True)
        sc.barrier()
        ar.reset(c_work)
        NJ = CH_C // 128
        xm = [ar.t("xm%d" % i, [128, D], F32) for i in range(2 * NJ)]
        sqC = ar.t("sqC", [128, D], BF16)
        hnC = ar.t("hnC", [128, D], F32)
        hbC = ar.t("hbC", [128, D], BF16)
        h2T = [ar.t("h2T%d" % i, [128, 8, CH_C], BF16) for i in range(2)]
        a1T = ar.t("a1T", [128, 32, CH_C], BF16)
        r32 = [ar.t("r32_%d" % i, [128, CH_C], F32) for i in range(2)]
        ytC = ar.t("ytC", [128, D], F32)
        xoC = [ar.t("xoC%d" % i, [128, D], F32) for i in range(2)]
        smC = ar.t("smC", [128, 16], F32)
        NCH = S // CH_C

        def loadC(cc):
            for j in range(NJ):
                tt = cc * NJ + j
                xi = (cc % 2) * NJ + j
                add("sp", [C("dma_start", out=xm[xi][:, :],
                                                               in_=x_mid[tt * 128:(tt + 1) * 128, :])],
                    writes=[("xm", xi)], dma=True)

        loadC(0)
        for cc in range(NCH):
            if cc + 1 < NCH:
                loadC(cc + 1)
            hb_ = cc % 2
            for j in range(NJ):
                xi = (cc % 2) * NJ + j
                add("act", C("activation", out=sqC[:, :], in_=xm[xi][:, :], func=AF.Square,
                                                         accum_out=smC[:, 0:1]),
                    reads=[("xm", xi)], writes=["sqC", "ssC"])
                rstd_from_ss(smC[:, 0:1], smC[:, 1:2], smC[:, 2:3], "rstdC", "ssC")
                add("pool", C("tensor_scalar", out=hnC[:, :], in0=xm[xi][:, :], scalar1=smC[:, 2:3],
                                                             scalar2=None, op0=ALU.mult),
                    reads=[("xm", xi), "rstdC"], writes=["hnC"])
                add("pool", C("tensor_tensor", out=hbC[:, :], in0=hnC[:, :], in1=gB1[:, :], op=ALU.mult),
                    reads=["hnC", "gBC1"], writes=["hbC"])
                pT = psb(7)
                for k in range(8):
                    add("pe", C("transpose", pT[:, k * 128:(k + 1) * 128],
                                                         hbC[:, k * 128:(k + 1) * 128], ident[:, :]),
                        reads=["hbC", "ident"], writes=[("bank", 7)])
                add("act", C("activation", out=h2T[hb_][:, :, j * 128:(j + 1) * 128],
                                                       in_=pT[:, :].rearrange("p (k t) -> p k t", k=8), func=AF.Copy),
                    reads=[("bank", 7)], writes=[("h2T", hb_)])
            for m in range(32):
                bk = m % 2
                for k in range(8):
                    add("pe", C("matmul",
                        PS[bk][:, 0:CH_C], w1b[:, k, m * 128:(m + 1) * 128], h2T[hb_][:, k, :],
                        start=(k == 0), stop=(k == 7)),
                        reads=[("h2T", hb_)], writes=[("bank", bk)])
                add("act", C("activation", out=r32[bk][:, :], in_=PS[bk][:, 0:CH_C], func=AF.Relu),
                    reads=[("bank", bk)], writes=[("r32", bk)])
                en = "dve" if m % 2 == 0 else "pool"
                add(en, C("tensor_tensor", out=a1T[:, m, :], in0=r32[bk][:, :], in1=r32[bk][:, :],
                                                             op=ALU.mult),
                    reads=[("r32", bk)], writes=[("a1T", m)])
            for j in range(NJ):
                tt = cc * NJ + j
                xi = (cc % 2) * NJ + j
                ob = tt % 2
                for n in range(2):
                    pg = PS[2 + 2 * (j % 2) + n]
                    for m in range(32):
                        add("pe", C("matmul",
                            pg[:, :], a1T[:, m, j * 128:(j + 1) * 128], w2b[:, m, n * 512:(n + 1) * 512],
                            start=(m == 0), stop=(m == 31)),
                            reads=[("a1T", m)], writes=[("bank", 2 + 2 * (j % 2) + n)])
                    add("act", C("activation", out=sqC[:, 0:512], in_=pg[:, :], func=AF.Square,
                                                                  accum_out=smC[:, 4 + n:5 + n]),
                        reads=[("bank", 2 + 2 * (j % 2) + n)], writes=["sqC", ("ssC2", n)])
                add("dve", C("tensor_tensor", out=smC[:, 6:7], in0=smC[:, 4:5], in1=smC[:, 5:6], op=ALU.add),
                    reads=[("ssC2", 0), ("ssC2", 1)], writes=["ssC2t"])
                rstd_from_ss(smC[:, 6:7], smC[:, 7:8], smC[:, 8:9], "rstdC2", "ssC2t")
                for n in range(2):
                    pg = PS[2 + 2 * (j % 2) + n]
                    add("dve", C("scalar_tensor_tensor",
                        out=ytC[:, n * 512:(n + 1) * 512], in0=pg[:, :], scalar=smC[:, 8:9],
                        in1=gB2[:, n * 512:(n + 1) * 512], op0=ALU.mult, op1=ALU.mult),
                        reads=[("bank", 2 + 2 * (j % 2) + n), "rstdC2", "gBC2"], writes=[("ytC", n)])
                add("pool", C("tensor_tensor", out=xoC[ob][:, :], in0=xm[xi][:, :],
                                                                    in1=ytC[:, :], op=ALU.add),
                    reads=[("xm", xi), ("ytC", 0), ("ytC", 1)], writes=[("xoC", ob)])
                add("sp", [C("dma_start", out=x_fin[tt * 128:(tt + 1) * 128, :],
                                                                 in_=xoC[ob][:, :])],
                    reads=[("xoC", ob)], writes=[("x_fin", tt)], dma=True)
        sc.barrier()

    sc.emit(nc)
    return nc


def _consts():
    bf = ml_dtypes.bfloat16
    ident = np.eye(128, dtype=np.float32).astype(bf)
    bands = np.zeros((128, 12, 128), np.float32)
    tp = np.arange(128)[:, None]
    t = np.arange(128)[None, :]
    for g, w in enumerate((2, 4, 8, 16)):
        cur = ((tp <= t) & (tp > t - w)).astype(np.float32) / w - (tp == t)
        prev = ((tp - 128 <= t) & (tp - 128 > t - w)).astype(np.float32) / w
        cnt = np.minimum(t + 1, w).astype(np.float32)
        cur0 = ((tp <= t) & (tp > t - w)).astype(np.float32) / cnt - (tp == t)
        bands[:, 3 * g + 0, :] = cur
        bands[:, 3 * g + 1, :] = prev
        bands[:, 3 * g + 2, :] = cur0
    half = 8
    inv = (np.float32(500000.0) ** (-np.arange(half, dtype=np.float32) / np.float32(half))).astype(np.float32)
    invf = np.broadcast_to(inv[None, :], (128, 8)).copy()
    return ident, bands.astype(bf), invf


_NC_CACHE = {}


def kernel(x, positions, g_pre_mix, w_in, w_pool, pool_scale, w_out, g_post_mix,
           g_pre_ffn, w_ff1, w_ff2, g_post_ffn):
    n = 8
    if "nc" not in _NC_CACHE:
        _NC_CACHE["nc"] = build_program(DEPTH)
    nc = _NC_CACHE["nc"]
    ident, bands, invf = _consts()
    f32 = np.float32
    shared = {
        "g_pre_mix": np.ascontiguousarray(g_pre_mix, f32),
        "w_in": np.ascontiguousarray(w_in, f32),
        "w_pool": np.ascontiguousarray(w_pool, f32),
        "pool_scale_t": np.ascontiguousarray(np.asarray(pool_scale, f32).reshape(DEPTH, 4, 128).transpose(0, 2, 1)),
        "w_out": np.ascontiguousarray(w_out, f32),
        "g_post_mix": np.ascontiguousarray(g_post_mix, f32),
        "g_pre_ffn": np.ascontiguousarray(g_pre_ffn, f32),
        "w_ff1": np.ascontiguousarray(w_ff1, f32),
        "w_ff2": np.ascontiguousarray(w_ff2, f32),
        "g_post_ffn": np.ascontiguousarray(g_post_ffn, f32),
        "c_ident": ident, "c_bands": bands, "c_invf": invf,
    }
    x = np.asarray(x, f32)
    positions = np.asarray(positions, np.int32)
    in_maps = []
    for b in range(n):
        m = dict(shared)
        m["x"] = np.ascontiguousarray(x[b])
        m["pos"] = np.ascontiguousarray(positions[b].reshape(NT, 128).T)
        in_maps.append(m)
    res = run_bass_kernel_spmd(nc, in_maps, core_ids=list(range(n)))
    out = np.stack([np.asarray(r["y"], f32) for r in res.results], axis=0)
    return out
```
